# Optimizing a Trainium2 kernel written in Bass

```python
import math
import jax, jax.numpy as jnp
from jax import lax
import numpy as np

D_MODEL = 1024
BATCH = 8
SEQ = 4096
DEPTH = 1

PLE_DIM = 256
D_MIX = D_MODEL
CONV_WIDTH = D_MIX // 2
CONV_HEADS = 8
POOL_WIDTH = D_MIX - CONV_WIDTH
POOL_WINDOWS = (2, 4, 8, 16)
POOL_GROUP = POOL_WIDTH // len(POOL_WINDOWS)
IN_PROJ_WIDTH = 3 * CONV_WIDTH + POOL_WIDTH
CONV_K = 3
PEER_HEADS = 8
PEER_QDIM = 256
PEER_HALF = PEER_QDIM // 2
N_KEYS = 128
N_EXPERTS = N_KEYS * N_KEYS
PEER_TOPK = 16
TOKEN_BLOCK = 128
EPS = 1e-6

kernel_name = "hybrid_conv_pool_peer_block"


def rms_norm(x, g):
    xf = x.astype(jnp.float32)
    y = xf * lax.rsqrt(jnp.mean(xf * xf, axis=-1, keepdims=True) + EPS)
    return (y * g.astype(jnp.float32)).astype(x.dtype)


def short_conv_mixer(b_gate, c_gate, v, conv_w):
    z = c_gate * v
    zp = jnp.pad(z, ((0, 0), (1, 1), (0, 0)))
    y = conv_w[0] * zp[:, :-2] + conv_w[1] * zp[:, 1:-1] + conv_w[2] * zp[:, 2:]
    return b_gate * y


def pool_mixer(u, pool_w, pool_scale):
    bsz, seq, _ = u.shape
    ug = u.astype(jnp.float32).reshape(bsz, seq, len(POOL_WINDOWS), POOL_GROUP)
    t = jnp.arange(seq)
    outs = []
    for g, w in enumerate(POOL_WINDOWS):
        xg = ug[:, :, g]
        cs = jnp.concatenate([jnp.zeros((bsz, 1, POOL_GROUP), jnp.float32),
                              jnp.cumsum(xg, axis=1)], axis=1)
        lo = jnp.clip(t - w // 2, 0, seq)
        hi = jnp.clip(t + w // 2, 0, seq)
        win_sum = jnp.take(cs, hi, axis=1) - jnp.take(cs, lo, axis=1)
        cnt = (hi - lo).astype(jnp.float32)[None, :, None]
        outs.append(win_sum / cnt - xg)
    pooled = jnp.stack(outs, axis=2).astype(u.dtype)
    y = jnp.einsum('bsgc,gcd->bsgd', pooled, pool_w)
    return y.reshape(bsz, seq, POOL_WIDTH) * pool_scale


def peer_block(xb, w_q, sub_keys, expert_u, expert_v):
    T = xb.shape[0]
    q = (xb @ w_q).reshape(T, PEER_HEADS, 2, PEER_HALF)
    s = jnp.einsum('thpd,hpkd->thpk', q, sub_keys)
    sv, si = lax.top_k(s, PEER_TOPK)
    cand = (sv[:, :, 0, :, None] + sv[:, :, 1, None, :]).reshape(T, PEER_HEADS, PEER_TOPK * PEER_TOPK)
    cidx = (si[:, :, 0, :, None] * N_KEYS + si[:, :, 1, None, :]).reshape(T, PEER_HEADS, PEER_TOPK * PEER_TOPK)
    top_s, pos = lax.top_k(cand, PEER_TOPK)
    eidx = jnp.take_along_axis(cidx, pos, axis=-1)
    gate = jax.nn.softmax(top_s.astype(jnp.float32), axis=-1).astype(xb.dtype)
    u = jnp.take(expert_u, eidx, axis=0)
    act = jax.nn.gelu(jnp.einsum('thkd,td->thk', u, xb))
    v = jnp.take(expert_v, eidx, axis=0)
    return jnp.einsum('thk,thkd->td', gate * act, v)


def peer_ffn(xn, w_q, sub_keys, expert_u, expert_v):
    bsz, seq, d = xn.shape
    blocks = xn.reshape(-1, TOKEN_BLOCK, d)
    out = lax.map(lambda xb: peer_block(xb, w_q, sub_keys, expert_u, expert_v), blocks)
    return out.reshape(bsz, seq, d)


def setup_inputs(seed: int = 0) -> dict:
    key = jax.random.key(seed)
    ks = jax.random.split(key, 20)
    f32 = jnp.float32
    def nrm(k, shape, scale):
        return jax.random.normal(k, shape, f32) * scale
    def gain(k, shape):
        return 1.0 + 0.05 * jax.random.normal(k, shape, f32)
    return {
        "x": nrm(ks[0], (BATCH, SEQ, D_MODEL), 1.0),
        "p": nrm(ks[1], (DEPTH, BATCH, SEQ, PLE_DIM), 1.0),
        "g_mix": gain(ks[2], (DEPTH, D_MODEL)),
        "w_in": nrm(ks[3], (DEPTH, D_MODEL, IN_PROJ_WIDTH), D_MODEL ** -0.5),
        "conv_w": nrm(ks[4], (DEPTH, CONV_K, CONV_WIDTH), CONV_K ** -0.5),
        "pool_w": nrm(ks[5], (DEPTH, len(POOL_WINDOWS), POOL_GROUP, POOL_GROUP), POOL_GROUP ** -0.5),
        "pool_scale": gain(ks[6], (DEPTH, POOL_WIDTH)),
        "w_o": nrm(ks[7], (DEPTH, D_MIX, D_MODEL), D_MIX ** -0.5),
        "g_ffn": gain(ks[8], (DEPTH, D_MODEL)),
        "w_q": nrm(ks[9], (DEPTH, D_MODEL, PEER_HEADS * PEER_QDIM), D_MODEL ** -0.5),
        "sub_keys": nrm(ks[10], (DEPTH, PEER_HEADS, 2, N_KEYS, PEER_HALF), PEER_HALF ** -0.5),
        "expert_u": nrm(ks[11], (DEPTH, N_EXPERTS, D_MODEL), D_MODEL ** -0.5),
        "expert_v": nrm(ks[12], (DEPTH, N_EXPERTS, D_MODEL), PEER_HEADS ** -0.5),
        "g_ple": gain(ks[13], (DEPTH, D_MODEL)),
        "w_ple_gate": nrm(ks[14], (DEPTH, D_MODEL, D_MODEL), D_MODEL ** -0.5),
        "w_ple_proj": nrm(ks[15], (DEPTH, PLE_DIM, D_MODEL), PLE_DIM ** -0.5),
        "g_final": gain(ks[16], (D_MODEL,)),
    }


def reference(x, p, g_mix, w_in, conv_w, pool_w, pool_scale, w_o, g_ffn, w_q, sub_keys,
              expert_u, expert_v, g_ple, w_ple_gate, w_ple_proj, g_final):
    h = x
    for i in range(DEPTH):
        xn = rms_norm(h, g_mix[i])
        proj = xn @ w_in[i]
        b_gate = proj[..., :CONV_WIDTH]
        c_gate = proj[..., CONV_WIDTH:2 * CONV_WIDTH]
        v_conv = proj[..., 2 * CONV_WIDTH:3 * CONV_WIDTH]
        u_pool = proj[..., 3 * CONV_WIDTH:]
        y_conv = short_conv_mixer(b_gate, c_gate, v_conv, conv_w[i])
        y_pool = pool_mixer(u_pool, pool_w[i], pool_scale[i])
        h = h + jnp.concatenate([y_conv, y_pool], axis=-1) @ w_o[i]
        h = h + peer_ffn(rms_norm(h, g_ffn[i]), w_q[i], sub_keys[i], expert_u[i], expert_v[i])
        gate = jax.nn.sigmoid(rms_norm(h, g_ple[i]) @ w_ple_gate[i])
        h = h + gate * (p[i] @ w_ple_proj[i])
    return rms_norm(h, g_final)
```

```python
import numpy as np
import ml_dtypes
import concourse.bass as bass
import concourse.mybir as mybir
from concourse.bass_utils import run_bass_kernel_spmd

F32 = mybir.dt.float32
BF16 = mybir.dt.bfloat16
U32 = mybir.dt.uint32
I32 = mybir.dt.int32
ALU = mybir.AluOpType
AF = mybir.ActivationFunctionType
AX = mybir.AxisListType

D = 1024
SEQ = 4096
NCORES = 8
TB = 256
HALO = 8
TW = TB + 2 * HALO
EPS = 1e-6
NEG = -3.0e38

DMA_SLOTS = {"sp": 8, "act": 4, "pool": 4}
STRICT_WAR = True


class Tile:
    __slots__ = ("t", "lw", "rd", "name")

    def __init__(self, t, name=""):
        self.t = t
        self.lw = None
        self.rd = []
        self.name = name

    def __getitem__(self, k):
        return self.t[k]


class Sched:
    def __init__(self, nc, sems):
        self.nc = nc
        self.sems = sems
        self.q = {e: [] for e in ("sp", "act", "dve", "pool", "pe")}
        self.cnt = {e: 0 for e in self.q}
        self.ndma = {e: 0 for e in DMA_SLOTS}
        self.seen = {e: {} for e in self.q}

    def _deps(self, eng, reads, writes):
        deps = []
        for t in reads:
            if t.lw is not None:
                deps.append(t.lw)
        for t in writes:
            if t.lw is not None:
                deps.append(t.lw)
            for r in t.rd:
                if r[0] == "c" and r[1] == eng and not STRICT_WAR:
                    continue
                deps.append(r)
        return deps

    def _waits(self, eng, deps):
        w = {}
        for d in deps:
            if d[0] == "c":
                _, y, n = d
                if y == eng and eng == "pe":
                    continue
                key = ("c", y)
                val = n
            else:
                _, qn, n = d
                k = DMA_SLOTS[qn]
                key = ("d", qn, n % k)
                val = 16 * (n // k + 1)
            if self.seen[eng].get(key, 0) >= val:
                continue
            if w.get(key, 0) < val:
                w[key] = val
        for key, v in w.items():
            self.seen[eng][key] = v
        return [(self.sems[key], v) for key, v in w.items()]

    def _mark(self, tok, reads, writes):
        for t in reads:
            t.rd.append(tok)
        for t in writes:
            t.lw = tok
            t.rd = []

    def op(self, eng, fn, reads=(), writes=(), skip_same=False):
        deps = self._deps(eng, reads, writes)
        if skip_same:
            deps = [d for d in deps if not (d[0] == "c" and d[1] == eng)]
        waits = self._waits(eng, deps)
        self.cnt[eng] += 1
        tok = ("c", eng, self.cnt[eng])
        self._mark(tok, reads, writes)
        self.q[eng].append((waits, fn, (self.sems[("c", eng)], 1)))
        return tok

    def dma(self, qn, fn, reads=(), writes=()):
        n = self.ndma[qn]
        k = DMA_SLOTS[qn]
        deps = self._deps(qn, reads, writes)
        if n >= k:
            deps.append(("d", qn, n - k))
        waits = self._waits(qn, deps)
        self.ndma[qn] += 1
        tok = ("d", qn, n)
        self._mark(tok, reads, writes)
        self.q[qn].append((waits, fn, (self.sems[("d", qn, n % k)], 16)))
        return tok

    def wait_all(self, eng, toks):
        waits = self._waits(eng, list(toks))
        if waits:
            self.q[eng].append((waits, None, None))

    def drain(self):
        toks = []
        for qn, k in DMA_SLOTS.items():
            n = self.ndma[qn]
            toks.extend(("d", qn, i) for i in range(max(0, n - k), n))
        self.wait_all("sp", toks)

    def flush(self, block):
        self.drain()
        for name, method in (("sp", block.sync), ("act", block.scalar), ("dve", block.vector),
                             ("pool", block.gpsimd), ("pe", block.tensor)):
            ops = self.q[name]
            self.q[name] = []
            if not ops:
                continue

            def body(e, ops=ops):
                for waits, fn, inc in ops:
                    for sem, val in waits:
                        e.wait_ge(sem, val)
                    if fn is None:
                        continue
                    ins = fn(e)
                    ins.then_inc(inc[0], inc[1])
            method(body)


def _alloc_sems(nc, stack):
    sems = {}
    for e in ("act", "dve", "pool", "pe"):
        sems[("c", e)] = stack.enter_context(nc.semaphore("c_" + e))
    for qn, k in DMA_SLOTS.items():
        for i in range(k):
            sems[("d", qn, i)] = stack.enter_context(nc.semaphore("d_%s%d" % (qn, i)))
    return sems


CAST_W = 4096


def _cast_block(nc, S, src, dst, nelem, tag):
    from contextlib import ExitStack
    npieces = nelem // CAST_W
    with ExitStack() as st:
        NBUF = 3
        fin = [Tile(st.enter_context(nc.sbuf_tensor("cin%s%d" % (tag, i), [128, CAST_W], F32))) for i in range(NBUF)]
        fout = [Tile(st.enter_context(nc.sbuf_tensor("cout%s%d" % (tag, i), [128, CAST_W], BF16))) for i in range(NBUF)]
        with nc.Block() as block:
            for pc in range(npieces):
                b = pc % NBUF
                sl = slice(pc * CAST_W, (pc + 1) * CAST_W)
                S.dma("sp", lambda e, b=b, sl=sl: e.dma_start(out=fin[b][:], in_=src[:, sl]),
                      reads=[src], writes=[fin[b]])
                eng = ("dve", "act", "pool")[pc % 3] if False else ("dve", "act")[pc % 2]
                if eng == "act":
                    S.op("act", lambda e, b=b: e.copy(out=fout[b][:], in_=fin[b][:]), reads=[fin[b]], writes=[fout[b]])
                else:
                    S.op(eng, lambda e, b=b: e.tensor_copy(out=fout[b][:], in_=fin[b][:]), reads=[fin[b]], writes=[fout[b]])
                S.dma("act", lambda e, b=b, sl=sl: e.dma_start(out=dst[:, sl], in_=fout[b][:]),
                      reads=[fout[b]], writes=[dst])
            S.flush(block)


def _views(tile_, n):
    return [Tile(tile_.t if isinstance(tile_, Tile) else tile_) for _ in range(n)]


class Ctx:
    pass


def _rmsnorm(nc, S, C, src, g_sb, dst, width, ps, sq, rs, tagw=None):
    for c in range(8):
        eng = "act" if c % 2 == 0 else "pool"
        if eng == "act":
            S.op("act", lambda e, c=c: e.activation(out=sq[:, c, 0:width], in_=src[:, c, 0:width], func=AF.Square),
                 reads=[src], writes=[sq])
        else:
            S.op("pool", lambda e, c=c: e.tensor_tensor(out=sq[:, c, 0:width], in0=src[:, c, 0:width],
                                                        in1=src[:, c, 0:width], op=ALU.mult),
                 reads=[src], writes=[sq])

    def mm(e):
        ins = None
        for c in range(8):
            ins = e.matmul(ps[:, 0:width], lhsT=C.ones[:, :], rhs=sq[:, c, 0:width], start=(c == 0), stop=(c == 7))
        return ins
    S.op("pe", mm, reads=[sq, C.ones], writes=[ps])
    S.op("act", lambda e: e.activation(out=rs[:, 0:width], in_=ps[:, 0:width], func=AF.Sqrt,
                                       bias=C.epsb[:, 0:1], scale=1.0 / D),
         reads=[ps, C.epsb], writes=[rs])
    S.op("dve", lambda e: e.reciprocal(out=rs[:, 0:width], in_=rs[:, 0:width]), reads=[rs], writes=[rs])
    for c in range(8):
        S.op("dve", lambda e, c=c: e.scalar_tensor_tensor(out=dst[:, c, 0:width], in0=src[:, c, 0:width],
                                                          scalar=g_sb[:, c:c + 1], in1=rs[:, 0:width],
                                                          op0=ALU.mult, op1=ALU.mult),
             reads=[src, g_sb, rs], writes=[dst])


def _load_cast(S, stg, dst, dst_sl, src, src_sl, width, i):
    st_ = stg[i % len(stg)]
    S.dma("sp", lambda e: e.dma_start(out=st_[:, 0:width], in_=src[src_sl]), reads=[src], writes=[st_])
    if i % 2 == 0:
        S.op("dve", lambda e: e.tensor_copy(out=dst[dst_sl], in_=st_[:, 0:width]), reads=[st_], writes=[dst])
    else:
        S.op("act", lambda e: e.copy(out=dst[dst_sl], in_=st_[:, 0:width]), reads=[st_], writes=[dst])


def build_program(nc, nb, debug=False):
    from contextlib import ExitStack
    seq = nb * TB
    okind = "ExternalOutput" if debug else "Internal"

    def din(name, shape, dt=F32):
        return Tile(nc.dram_tensor(name, shape, dt, kind="ExternalInput").ap(), name)

    xT = din("xT", [D, seq])
    pT = din("pT", [256, seq])
    w_in = din("w_in", [D, 2048])
    w_o = din("w_o", [D, D])
    w_q = din("w_q", [D, 2048])
    w_pg = din("w_pg", [D, D])
    w_pp = din("w_pp", [256, D])
    keysT = din("keysT", [128, 2048])
    poolw = din("poolw", [128, 512])
    small = din("small", [128, 48])
    UT = din("UT", [128, 131072])
    VV = din("VV", [128, 131072])
    ident_d = din("ident", [128, 128])
    edge_d = din("edge", [128, 64])
    iota_d = din("iota", [128, 128])
    bits_d = din("bits", [128, 256], U32)
    outT = Tile(nc.dram_tensor("outT", [D, seq], F32, kind="ExternalOutput").ap(), "outT")
    UTb = nc.dram_tensor("UTb", [128, 131072], BF16, kind="Internal").ap()
    VVb = nc.dram_tensor("VVb", [128, 131072], BF16, kind="Internal").ap()
    h1T = nc.dram_tensor("h1T", [D, seq], F32, kind=okind).ap()
    xn2T = nc.dram_tensor("xn2T", [D, seq], BF16, kind=okind).ap()
    ijg = nc.dram_tensor("ijg", [128, 3, seq], BF16, kind=okind).ap()
    NPIECE = 131072 // CAST_W
    UTb_t = _views(UTb, NPIECE)
    VVb_t = _views(VVb, NPIECE)
    h1T_t = _views(h1T, nb)
    xn2T_t = _views(xn2T, nb)
    ijg_t = _views(ijg, nb)

    xT_v = xT.t.rearrange("(c p) t -> p c t", p=128)
    pT_v = pT.t.rearrange("(c p) t -> p c t", p=128)
    outT_v = outT.t.rearrange("(c p) t -> p c t", p=128)
    h1T_v = h1T.rearrange("(c p) t -> p c t", p=128)
    xn2T_v = xn2T.rearrange("(c p) t -> p c t", p=128)

    with ExitStack() as top:
        sems = _alloc_sems(nc, top)
        S = Sched(nc, sems)


        _phase1(nc, S, nb, locals())

        _phase2(nc, S, nb, locals())
    return nc


def _cast_tables(nc, S, UT, UTb_t, VV, VVb_t):
    from contextlib import ExitStack
    with ExitStack() as st:
        NBUF = 3
        fin = [Tile(st.enter_context(nc.sbuf_tensor("cin%d" % i, [128, CAST_W], F32))) for i in range(NBUF)]
        fout = [Tile(st.enter_context(nc.sbuf_tensor("cout%d" % i, [128, CAST_W], BF16))) for i in range(NBUF)]
        with nc.Block() as block:
            k = 0
            for src, dst_t in ((UT, UTb_t), (VV, VVb_t)):
                for pc in range(len(dst_t)):
                    b = k % NBUF
                    sl = slice(pc * CAST_W, (pc + 1) * CAST_W)
                    S.dma("sp", lambda e, b=b, sl=sl, src=src: e.dma_start(out=fin[b][:], in_=src[:, sl]),
                          reads=[src], writes=[fin[b]])
                    eng = ("dve", "act", "pool")[k % 3]
                    if eng == "act":
                        S.op("act", lambda e, b=b: e.copy(out=fout[b][:], in_=fin[b][:]),
                             reads=[fin[b]], writes=[fout[b]])
                    else:
                        S.op(eng, lambda e, b=b: e.tensor_copy(out=fout[b][:], in_=fin[b][:]),
                             reads=[fin[b]], writes=[fout[b]])
                    d = dst_t[pc]
                    S.dma("act", lambda e, b=b, sl=sl, d=d: e.dma_start(out=d[:, sl], in_=fout[b][:]),
                          reads=[fout[b]], writes=[d])
                    k += 1
            S.flush(block)


def _nop():
    pass


def _merge(lists):
    lists = [l for l in lists if l]
    idx = [0] * len(lists)
    out = []
    total = sum(len(l) for l in lists)
    for _ in range(total):
        best = None
        for i, l in enumerate(lists):
            if idx[i] < len(l):
                frac = (idx[i] + 0.5) / len(l)
                if best is None or frac < best[0]:
                    best = (frac, i)
        i = best[1]
        out.append(lists[i][idx[i]])
        idx[i] += 1
    return out


def _phase1(nc, S, nb, G):
    from contextlib import ExitStack
    xT_v, h1T_v, xn2T_v, ijg = G["xT_v"], G["h1T_v"], G["xn2T_v"], G["ijg"]
    xT, h1T_t, xn2T_t, ijg_t = G["xT"], G["h1T_t"], G["xn2T_t"], G["ijg_t"]
    seq = nb * TB
    CW2 = 2048
    with ExitStack() as st:
        def sb(name, shape, dt):
            return Tile(st.enter_context(nc.sbuf_tensor("a_" + name, shape, dt)), name)

        def psum(name):
            return Tile(st.enter_context(nc.psum_tensor("a_" + name, [128, 512], F32)), name)

        C = Ctx()
        w_in_sb = sb("w_in", [128, 8, 2048], BF16)
        w_o_sb = sb("w_o", [128, 8, 1024], BF16)
        w_q_sb = sb("w_q", [128, 8, 2048], BF16)
        keys_sb = sb("keys", [128, 16, 128], BF16)
        poolw_sb = sb("poolw", [128, 4, 128], BF16)
        small_sb = sb("small", [128, 48], F32)
        ident = sb("ident", [128, 128], F32)
        C.ones = sb("ones", [128, 128], BF16)
        C.epsb = sb("epsb", [128, 1], F32)
        iota16 = sb("iota16", [128, 16], F32)
        bits_sb = sb("bits", [128, 256], U32)
        edge = sb("edge", [128, 2, 4, 8], F32)
        stg = [sb("stg%d" % i, [128, CW2], F32) for i in range(2)]
        cout = [sb("cout%d" % i, [128, CW2], BF16) for i in range(2)]

        xt = sb("xt", [128, 8, TW], F32)
        sq = sb("sq", [128, 8, TW], BF16)
        rs = sb("rs", [128, TW], F32)
        xn = sb("xn", [128, 8, TW], BF16)
        bsb = sb("bsb", [128, 4, TB], BF16)
        z = sb("z", [128, 4, TW], F32)
        usb = sb("usb", [128, 4, TW], F32)
        pa_ = sb("pa", [128, TW], F32)
        pb_ = sb("pb", [128, TW], F32)
        tcv = sb("tcv", [128, 4, TB], F32)
        tcv_v = _views(tcv, 4)
        pooled = sb("pooled", [128, 4, TB], BF16)
        xn2_ = [sb("xn2_%d" % i, [128, 8, TB], BF16) for i in range(2)]
        qT = [sb("qT%d" % i, [128, 16, TB], BF16) for i in range(2)]
        scrA = sb("scrA", [128, 2048], F32)
        cand = sb("cand", [128, 8, 256], F32)
        sv = sb("sv", [128, 16, 16], F32)
        si = sb("si", [128, 16, 16], U32)
        sif = sb("sif", [128, 16, 16], F32)
        top = sb("top", [128, 8, 16], F32)
        pos = sb("pos", [128, 8, 16], U32)
        ex = sb("ex", [128, 8, 16], F32)
        zs = sb("zs", [128, 8], F32)
        gg = sb("gg", [128, 8, 16], F32)
        au = sb("au", [128, 8, 16], U32)
        bu = sb("bu", [128, 8, 16], U32)
        af = sb("af", [128, 8, 16], F32)
        bf = sb("bf", [128, 8, 16], F32)
        IJ = sb("IJ", [128, 2, 128], F32)
        ijg_sb = sb("ijg_sb", [128, 3, TB], BF16)
        P = [psum("p%d" % i) for i in range(8)]

        sA_v = _views(scrA, 8)
        sB_v = _views(scrA, 8)
        scrA_all = sA_v + sB_v
        sv_v = _views(sv, 16)
        si_v = _views(si, 16)
        cand_v = _views(cand, 8)
        top_v = _views(top, 8)
        pos_v = _views(pos, 8)
        s_sb = scrA.t[:, 0:1024].rearrange("p (q k) -> p q k", k=128)
        s_u = scrA.t[:, 0:1024].bitcast(U32).rearrange("p (q k) -> p q k", k=128)
        s2 = scrA.t[:, 1024:2048].rearrange("p (q k) -> p q k", k=128)
        oh = scrA.t[:, :].rearrange("p (s a) -> p s a", a=16)
        ycat = xn.t[:, :, 0:TB]
        h1 = xt.t[:, :, HALO:HALO + TB]

        gmix = Tile(small_sb.t[:, 0:8])
        gffn = Tile(small_sb.t[:, 8:16])
        convw = small_sb.t[:, 32:44].rearrange("p (k t) -> p k t", t=3)
        pscale = small_sb.t[:, 44:48]

        pj_i = [0]

        def pj():
            pj_i[0] += 1
            return (P[1], P[2], P[3], P[7])[pj_i[0] % 4]

        def norm_steps(src_t, src_ap, g_sb, dst_t, dst_ap, width):
            steps = []

            def s1():
                for c in range(8):
                    S.op("act", lambda e, c=c: e.activation(out=sq[:, c, 0:width], in_=src_ap(c), func=AF.Square), reads=[src_t], writes=[sq])
            steps.append(s1)
            steps.extend([_nop] * 2)

            def s2_():
                def mm(e):
                    ins = None
                    for c in range(8):
                        ins = e.matmul(P[0][:, 0:width], lhsT=C.ones[:, :], rhs=sq[:, c, 0:width], start=(c == 0), stop=(c == 7))
                    return ins
                S.op("pe", mm, reads=[sq, C.ones], writes=[P[0]])
                S.op("act", lambda e: e.activation(out=rs[:, 0:width], in_=P[0][:, 0:width], func=AF.Sqrt, bias=C.epsb[:, 0:1], scale=1.0 / D),
                     reads=[P[0], C.epsb], writes=[rs])
                S.op("dve", lambda e: e.reciprocal(out=rs[:, 0:width], in_=rs[:, 0:width]), reads=[rs], writes=[rs])
            steps.append(s2_)
            steps.extend([_nop] * 3)

            def s3():
                for c in range(8):
                    S.op("dve", lambda e, c=c: e.scalar_tensor_tensor(out=dst_ap(c), in0=src_ap(c), scalar=g_sb[:, c:c + 1], in1=rs[:, 0:width],
                                                                      op0=ALU.mult, op1=ALU.mult), reads=[src_t, g_sb, rs], writes=[dst_t])
            steps.append(s3)
            return steps

        def mixer_steps(b):
            steps = []
            t0 = b * TB
            lo = max(t0 - HALO, 0)
            hi = min(t0 + TB + HALO, seq)
            c_lo = lo - (t0 - HALO)
            c_hi = hi - (t0 - HALO)
            xn2 = xn2_[b % 2]

            def load():
                if c_lo > 0:
                    S.op("pool", lambda e: e.memset(xt[:, :, 0:HALO], 0.0), writes=[xt])
                if c_hi < TW:
                    S.op("pool", lambda e: e.memset(xt[:, :, TW - HALO:TW], 0.0), writes=[xt])
                S.dma("sp", lambda e: e.dma_start(out=xt[:, :, c_lo:c_hi], in_=xT_v[:, :, lo:hi]), reads=[xT], writes=[xt])
            steps.append(load)
            steps.extend(norm_steps(xt, lambda c: xt[:, c, 0:TW], gmix, xn, lambda c: xn[:, c, 0:TW], TW))

            def inproj(oc):
                pp = pj()

                def mm(e):
                    ins = None
                    for c in range(8):
                        ins = e.matmul(pp[:, 0:TW], lhsT=w_in_sb[:, c, oc * 128:(oc + 1) * 128], rhs=xn[:, c, :], start=(c == 0), stop=(c == 7))
                    return ins
                S.op("pe", mm, reads=[w_in_sb, xn], writes=[pp])
                kk = oc % 4
                if oc < 4:
                    S.op("act", lambda e: e.copy(out=bsb[:, kk, :], in_=pp[:, HALO:HALO + TB]), reads=[pp], writes=[bsb])
                elif oc < 8:
                    S.op("act", lambda e: e.copy(out=z[:, kk, :], in_=pp[:, 0:TW]), reads=[pp], writes=[z])
                elif oc < 12:
                    S.op("dve", lambda e: e.tensor_tensor(out=z[:, kk, :], in0=pp[:, 0:TW], in1=z[:, kk, :], op=ALU.mult), reads=[pp, z], writes=[z])
                else:
                    S.op("act", lambda e: e.copy(out=usb[:, kk, :], in_=pp[:, 0:TW]), reads=[pp], writes=[usb])
            ip_order = [12, 13, 14, 15, 4, 5, 6, 7, 8, 9, 10, 11, 0, 1, 2, 3]
            for n_, oc in enumerate(ip_order):
                steps.append(lambda oc=oc: inproj(oc))
                if 4 <= n_ < 8:
                    steps.append(lambda g=n_ - 4: pool(g))

            def conv(step):
                for kk in range(4):
                    tv = tcv_v[kk]
                    if step == 0:
                        S.op("dve", lambda e, kk=kk: e.tensor_scalar(out=tcv[:, kk, :], in0=z[:, kk, HALO:HALO + TB], scalar1=convw[:, kk, 1:2],
                                                                     scalar2=None, op0=ALU.mult), reads=[z, small_sb], writes=[tv])
                    elif step == 1:
                        S.op("dve", lambda e, kk=kk: e.scalar_tensor_tensor(out=tcv[:, kk, :], in0=z[:, kk, HALO - 1:HALO - 1 + TB], scalar=convw[:, kk, 0:1],
                                                                            in1=tcv[:, kk, :], op0=ALU.mult, op1=ALU.add), reads=[z, tv, small_sb], writes=[tv])
                    elif step == 2:
                        S.op("dve", lambda e, kk=kk: e.scalar_tensor_tensor(out=tcv[:, kk, :], in0=z[:, kk, HALO + 1:HALO + 1 + TB], scalar=convw[:, kk, 2:3],
                                                                            in1=tcv[:, kk, :], op0=ALU.mult, op1=ALU.add), reads=[z, tv, small_sb], writes=[tv])
                    else:
                        S.op("dve", lambda e, kk=kk: e.tensor_tensor(out=ycat[:, kk, :], in0=tcv[:, kk, :], in1=bsb[:, kk, :], op=ALU.mult),
                             reads=[tv, bsb], writes=[xn])
            for st_ in range(4):
                steps.append(lambda st_=st_: conv(st_))

            def pool(g):
                w = 2 << g
                cur = (usb, lambda sl: usb[:, g, sl])
                width = TW
                bufs = [pa_, pb_]
                for si_, step in enumerate((1, 2, 4, 8)[:g + 1]):
                    nxt_t = bufs[si_ % 2]
                    width2 = width - step
                    ct, cf = cur
                    S.op("pool", lambda e, cf=cf, nxt_t=nxt_t, width2=width2, step=step: e.tensor_tensor(
                        out=nxt_t[:, 0:width2], in0=cf(slice(0, width2)), in1=cf(slice(step, step + width2)), op=ALU.add),
                        reads=[ct], writes=[nxt_t])
                    cur = (nxt_t, lambda sl, nxt_t=nxt_t: nxt_t[:, sl])
                    width = width2
                ct, cf = cur
                off = HALO - w // 2
                edges = []
                if b == 0:
                    edges.append((0, 0))
                if b == nb - 1:
                    edges.append((1, TB - 8))
                for (ei, c0) in edges:
                    S.op("pool", lambda e, ei=ei, c0=c0: e.tensor_tensor(
                        out=cf(slice(off + c0, off + c0 + 8)), in0=cf(slice(off + c0, off + c0 + 8)), in1=edge[:, ei, g, :], op=ALU.mult),
                        reads=[ct, edge], writes=[ct])
                S.op("dve", lambda e: e.scalar_tensor_tensor(
                    out=pooled[:, g, :], in0=cf(slice(off, off + TB)), scalar=1.0 / w, in1=usb[:, g, HALO:HALO + TB],
                    op0=ALU.mult, op1=ALU.subtract), reads=[ct, usb], writes=[pooled])

            def pool_back(g):
                pp = pj()
                S.op("pe", lambda e: e.matmul(pp[:, 0:TB], lhsT=poolw_sb[:, g, :], rhs=pooled[:, g, :], start=True, stop=True),
                     reads=[poolw_sb, pooled], writes=[pp])
                S.op("act", lambda e: e.activation(out=ycat[:, 4 + g, :], in_=pp[:, 0:TB], func=AF.Copy, scale=pscale[:, g:g + 1]),
                     reads=[pp, small_sb], writes=[xn])
            for g in range(4):
                steps.append(lambda g=g: pool_back(g))

            def outproj(oc):
                pp = pj()

                def mm(e):
                    ins = None
                    for c in range(8):
                        ins = e.matmul(pp[:, 0:TB], lhsT=w_o_sb[:, c, oc * 128:(oc + 1) * 128], rhs=ycat[:, c, :], start=(c == 0), stop=(c == 7))
                    return ins
                S.op("pe", mm, reads=[w_o_sb, xn], writes=[pp])
                S.op("dve", lambda e: e.tensor_tensor(out=h1[:, oc, :], in0=pp[:, 0:TB], in1=h1[:, oc, :], op=ALU.add),
                     reads=[pp, xt], writes=[xt])
                S.op("act", lambda e: e.activation(out=sq[:, oc, 0:TB], in_=h1[:, oc, :], func=AF.Square), reads=[xt], writes=[sq])
            for oc in range(8):
                steps.append(lambda oc=oc: outproj(oc))
            steps.append(lambda: S.dma("sp", lambda e: e.dma_start(out=h1T_v[:, :, t0:t0 + TB], in_=h1), reads=[xt], writes=[h1T_t[b]]))
            steps.extend(norm_steps(xt, lambda c: h1[:, c, :], gffn, xn2, lambda c: xn2[:, c, :], TB)[3:])
            steps.append(lambda: S.dma("sp", lambda e: e.dma_start(out=xn2T_v[:, :, t0:t0 + TB], in_=xn2[:, :, :]), reads=[xn2], writes=[xn2T_t[b]]))

            return steps

        def q_steps(b):
            steps = []
            xn2 = xn2_[b % 2]
            qTb = qT[b % 2]

            def qproj(qc):
                pp = pj()

                def mm(e):
                    ins = None
                    for c in range(8):
                        ins = e.matmul(pp[:, 0:TB], lhsT=w_q_sb[:, c, qc * 128:(qc + 1) * 128], rhs=xn2[:, c, :], start=(c == 0), stop=(c == 7))
                    return ins
                S.op("pe", mm, reads=[w_q_sb, xn2], writes=[pp])
                S.op("act", lambda e: e.copy(out=qTb[:, qc, :], in_=pp[:, 0:TB]), reads=[pp], writes=[qTb])
            for qc in range(16):
                steps.append(lambda qc=qc: qproj(qc))
            return steps

        def routing_steps(b):
            steps = []
            t0 = b * TB
            qTb = qT[b % 2]

            def scores(tsl, half):
                for q4 in range(2):
                    pp = P[4 + q4]

                    def mm(e, pp=pp, q4=q4):
                        ins = None
                        for i in range(4):
                            qc = half * 8 + q4 * 4 + i
                            ins = e.matmul(pp[:, i * 128:(i + 1) * 128], lhsT=qTb[:, qc, tsl], rhs=keys_sb[:, qc, :], start=True, stop=True)
                        return ins
                    S.op("pe", mm, reads=[qTb, keys_sb], writes=[pp])
                    S.op("act", lambda e, pp=pp, q4=q4: e.copy(out=s_sb[:, q4 * 4:(q4 + 1) * 4, :], in_=pp[:, :].rearrange("p (q k) -> p q k", k=128)),
                         reads=[pp], writes=sA_v[q4 * 4:(q4 + 1) * 4])
                S.op("dve", lambda e: e.tensor_tensor(out=s_u, in0=s_u, in1=bits_sb[:, 0:128].unsqueeze(1).broadcast_to([128, 8, 128]),
                                                      op=ALU.bitwise_and), reads=sA_v + [bits_sb], writes=sA_v)
                S.op("dve", lambda e: e.tensor_tensor(out=s_u, in0=s_u, in1=bits_sb[:, 128:256].unsqueeze(1).broadcast_to([128, 8, 128]),
                                                      op=ALU.bitwise_or), reads=sA_v + [bits_sb], writes=sA_v)

            def topk_step(half, step):
                for q in range(8):
                    qc = half * 8 + q
                    if step == 0:
                        S.op("dve", lambda e, q=q, qc=qc: e.max(out=sv[:, qc, 0:8], in_=s_sb[:, q, :]), reads=[sA_v[q]], writes=[sv_v[qc]])
                    elif step == 1:
                        S.op("dve", lambda e, q=q, qc=qc: e.match_replace(out=s2[:, q, :], in_to_replace=sv[:, qc, 0:8], in_values=s_sb[:, q, :], imm_value=NEG),
                             reads=[sA_v[q], sv_v[qc]], writes=[sB_v[q]])
                    elif step == 2:
                        S.op("dve", lambda e, q=q, qc=qc: e.max(out=sv[:, qc, 8:16], in_=s2[:, q, :]), reads=[sB_v[q]], writes=[sv_v[qc]])
                    elif step == 3:
                        S.op("dve", lambda e, q=q, qc=qc: e.max_index(out=si[:, qc, 0:8], in_max=sv[:, qc, 0:8], in_values=s_sb[:, q, :]),
                             reads=[sA_v[q], sv_v[qc]], writes=[si_v[qc]])
                    else:
                        S.op("dve", lambda e, q=q, qc=qc: e.max_index(out=si[:, qc, 8:16], in_max=sv[:, qc, 8:16], in_values=s_sb[:, q, :]),
                             reads=[sA_v[q], sv_v[qc]], writes=[si_v[qc]])

            def idx_step():
                S.op("dve", lambda e: e.tensor_single_scalar(out=si[:, :, :], in_=sv.t[:, :, :].bitcast(U32), scalar=127, op=ALU.bitwise_and),
                     reads=sv_v, writes=si_v)

            def cand_step():
                sv4 = sv.t[:, :, :].rearrange("p (h two) k -> p h two k", two=2)
                S.op("dve", lambda e: e.tensor_tensor(
                    out=cand[:, :, :].rearrange("p h (a b) -> p h a b", b=16),
                    in0=sv4[:, :, 0, :].unsqueeze(3).broadcast_to([128, 8, 16, 16]),
                    in1=sv4[:, :, 1, :].unsqueeze(2).broadcast_to([128, 8, 16, 16]), op=ALU.add), reads=sv_v, writes=cand_v)

            def topk2_step(step):
                for h in range(8):
                    if step == 0:
                        S.op("dve", lambda e, h=h: e.max(out=top[:, h, 0:8], in_=cand[:, h, :]), reads=[cand_v[h]], writes=[top_v[h]])
                    elif step == 1:
                        S.op("dve", lambda e, h=h: e.max_index(out=pos[:, h, 0:8], in_max=top[:, h, 0:8], in_values=cand[:, h, :]),
                             reads=[cand_v[h], top_v[h]], writes=[pos_v[h]])
                    elif step == 2:
                        S.op("dve", lambda e, h=h: e.match_replace(out=cand[:, h, :], in_to_replace=top[:, h, 0:8], in_values=cand[:, h, :], imm_value=NEG),
                             reads=[cand_v[h], top_v[h], pos_v[h]], writes=[cand_v[h]])
                    elif step == 3:
                        S.op("dve", lambda e, h=h: e.max(out=top[:, h, 8:16], in_=cand[:, h, :]), reads=[cand_v[h]], writes=[top_v[h]])
                    else:
                        S.op("dve", lambda e, h=h: e.max_index(out=pos[:, h, 8:16], in_max=top[:, h, 8:16], in_values=cand[:, h, :]),
                             reads=[cand_v[h], top_v[h]], writes=[pos_v[h]])

            def softmax_step():
                S.op("dve", lambda e: e.tensor_tensor(out=ex[:, :, :], in0=top[:, :, :], in1=top[:, :, 0:1].broadcast_to([128, 8, 16]), op=ALU.subtract),
                     reads=top_v, writes=[ex])
                S.op("act", lambda e: e.activation(out=ex[:, :, :], in_=ex[:, :, :], func=AF.Exp), reads=[ex], writes=[ex])
                S.op("dve", lambda e: e.tensor_reduce(out=zs[:, :], in_=ex[:, :, :], axis=AX.X, op=ALU.add), reads=[ex], writes=[zs])
                S.op("dve", lambda e: e.reciprocal(out=zs[:, :], in_=zs[:, :]), reads=[zs], writes=[zs])
                S.op("dve", lambda e: e.tensor_tensor(out=gg[:, :, :], in0=ex[:, :, :], in1=zs[:, :].unsqueeze(2).broadcast_to([128, 8, 16]), op=ALU.mult),
                     reads=[ex, zs], writes=[gg])

            def decode_prep():
                S.op("dve", lambda e: e.tensor_single_scalar(out=au[:, :, :], in_=pos[:, :, :], scalar=4, op=ALU.arith_shift_right), reads=pos_v, writes=[au])
                S.op("dve", lambda e: e.tensor_single_scalar(out=bu[:, :, :], in_=pos[:, :, :], scalar=15, op=ALU.bitwise_and), reads=pos_v, writes=[bu])
                S.op("dve", lambda e: e.tensor_copy(out=af[:, :, :], in_=au[:, :, :]), reads=[au], writes=[af])
                S.op("dve", lambda e: e.tensor_copy(out=bf[:, :, :], in_=bu[:, :, :]), reads=[bu], writes=[bf])
                S.op("dve", lambda e: e.tensor_copy(out=sif[:, :, :], in_=si[:, :, :]), reads=si_v, writes=[sif])

            def decode(wi):
                sel, half_ = ((af, 0), (bf, 1))[wi]
                sif4 = sif.t[:, :, :].rearrange("p (h two) k -> p h two k", two=2)
                S.op("dve", lambda e: e.tensor_tensor(
                    out=oh[:, :, :], in0=sel[:, :, :].rearrange("p h k -> p (h k)").unsqueeze(2).broadcast_to([128, 128, 16]),
                    in1=iota16[:, :].unsqueeze(1).broadcast_to([128, 128, 16]), op=ALU.is_equal), reads=[sel, iota16], writes=scrA_all)
                S.op("dve", lambda e: e.tensor_tensor(
                    out=oh[:, :, :].rearrange("p (h k) a -> p h k a", k=16), in0=oh[:, :, :].rearrange("p (h k) a -> p h k a", k=16),
                    in1=sif4[:, :, half_, :].unsqueeze(2).broadcast_to([128, 8, 16, 16]), op=ALU.mult), reads=scrA_all + [sif], writes=scrA_all)
                S.op("dve", lambda e: e.tensor_reduce(out=IJ[:, wi, :], in_=oh[:, :, :], axis=AX.X, op=ALU.add), reads=scrA_all, writes=[IJ])

            def transp(tsl):
                def trp(e):
                    e.transpose(out=P[6][:, 0:128], in_=IJ[:, 0, :], identity=ident[:, :])
                    e.transpose(out=P[6][:, 128:256], in_=IJ[:, 1, :], identity=ident[:, :])
                    return e.transpose(out=P[6][:, 256:384], in_=gg[:, :, :].rearrange("p h k -> p (h k)"), identity=ident[:, :])
                S.op("pe", trp, reads=[IJ, gg, ident], writes=[P[6]])
                S.op("act", lambda e: e.copy(out=ijg_sb[:, :, tsl], in_=P[6][:, 0:384].rearrange("p (a t) -> p a t", t=128)),
                     reads=[P[6]], writes=[ijg_sb])

            for ts_ in range(2):
                tsl = slice(ts_ * 128, (ts_ + 1) * 128)
                for half in range(2):
                    steps.append(lambda tsl=tsl, half=half: scores(tsl, half))
                    for step in range(3):
                        steps.append(lambda half=half, step=step: topk_step(half, step))
                steps.append(idx_step)
                steps.append(cand_step)
                for step in range(5):
                    steps.append(lambda step=step: topk2_step(step))
                steps.append(softmax_step)
                steps.append(decode_prep)
                steps.append(lambda: decode(0))
                steps.append(lambda: decode(1))
                steps.append(lambda tsl=tsl: transp(tsl))
            steps.append(lambda: S.dma("sp", lambda e: e.dma_start(out=ijg[:, :, t0:t0 + TB], in_=ijg_sb[:, :, :]), reads=[ijg_sb], writes=[ijg_t[b]]))
            return steps

        def cast_steps():
            pieces = []
            for src, dst_t, dst in ((G["UT"], G["UTb_t"], G["UTb"]), (G["VV"], G["VVb_t"], G["VVb"])):
                for pc in range(131072 // CW2):
                    pieces.append((src, dst_t, dst, pc))

            def load(i):
                src, dst_t, dst, pc = pieces[i]
                fi = stg[i % 2]
                sl = slice(pc * CW2, (pc + 1) * CW2)
                S.dma("sp", lambda e: e.dma_start(out=fi[:, :], in_=src[:, sl]), reads=[src], writes=[fi])

            def piece(i):
                if i == 0:
                    load(0)
                if i + 1 < len(pieces):
                    load(i + 1)
                src, dst_t, dst, pc = pieces[i]
                fi = stg[i % 2]
                fo = cout[i % 2]
                sl = slice(pc * CW2, (pc + 1) * CW2)
                S.op("act", lambda e: e.copy(out=fo[:, :], in_=fi[:, :]), reads=[fi], writes=[fo])
                d = dst_t[(pc * CW2) // CAST_W]
                S.dma("act", lambda e: e.dma_start(out=dst[:, sl], in_=fo[:, :]), reads=[fo], writes=[d])
            return [(lambda i=i: piece(i)) for i in range(len(pieces))]

        with nc.Block() as block:
            S.dma("sp", lambda e: e.dma_start(out=small_sb[:], in_=G["small"][:]), reads=[G["small"]], writes=[small_sb])
            gmix.lw = gffn.lw = small_sb.lw
            S.dma("sp", lambda e: e.dma_start(out=ident[:], in_=G["ident_d"][:]), reads=[G["ident_d"]], writes=[ident])
            S.dma("sp", lambda e: e.dma_start(out=iota16[:], in_=G["iota_d"][:, 0:16]), reads=[G["iota_d"]], writes=[iota16])
            S.dma("sp", lambda e: e.dma_start(out=bits_sb[:], in_=G["bits_d"][:, :]), reads=[G["bits_d"]], writes=[bits_sb])
            S.dma("sp", lambda e: e.dma_start(out=edge[:], in_=G["edge_d"][:, :].rearrange("p (a g c) -> p a g c", a=2, g=4)),
                  reads=[G["edge_d"]], writes=[edge])
            S.op("pool", lambda e: e.memset(C.ones[:], 1.0), writes=[C.ones])
            S.op("pool", lambda e: e.memset(C.epsb[:], EPS), writes=[C.epsb])
            k = 0
            for c in range(8):
                _load_cast(S, stg, w_in_sb, (slice(None), c, slice(None)), G["w_in"],
                           (slice(c * 128, (c + 1) * 128), slice(None)), 2048, k); k += 1
            for c in range(8):
                _load_cast(S, stg, w_o_sb, (slice(None), c, slice(None)), G["w_o"],
                           (slice(c * 128, (c + 1) * 128), slice(None)), 1024, k); k += 1
            for c in range(8):
                _load_cast(S, stg, w_q_sb, (slice(None), c, slice(None)), G["w_q"],
                           (slice(c * 128, (c + 1) * 128), slice(None)), 2048, k); k += 1
            for hh in range(2):
                stt = stg[k % 2]
                S.dma("sp", lambda e, stt=stt, hh=hh: e.dma_start(out=stt[:, 0:1024], in_=G["keysT"][:, hh * 1024:(hh + 1) * 1024]),
                      reads=[G["keysT"]], writes=[stt])
                S.op("dve", lambda e, stt=stt, hh=hh: e.tensor_copy(
                    out=keys_sb[:, hh * 8:(hh + 1) * 8, :], in_=stt[:, 0:1024].rearrange("p (q k) -> p q k", k=128)),
                    reads=[stt], writes=[keys_sb]); k += 1
            stt = stg[k % 2]
            S.dma("sp", lambda e, stt=stt: e.dma_start(out=stt[:, 0:512], in_=G["poolw"][:, :]), reads=[G["poolw"]], writes=[stt])
            S.op("dve", lambda e, stt=stt: e.tensor_copy(out=poolw_sb[:, :, :], in_=stt[:, 0:512].rearrange("p (g d) -> p g d", d=128)),
                 reads=[stt], writes=[poolw_sb]); k += 1

            casts = cast_steps()
            nseg = nb + 2
            per = (len(casts) + nseg - 1) // nseg
            cast_chunks = [casts[i * per:(i + 1) * per] for i in range(nseg)]
            for sg_ in range(nseg):
                lists = []
                if sg_ < nb:
                    lists.append(mixer_steps(sg_))
                if 0 <= sg_ - 1 < nb:
                    lists.append(q_steps(sg_ - 1))
                if 0 <= sg_ - 2 < nb:
                    lists.append(routing_steps(sg_ - 2))
                lists.append(cast_chunks[sg_])
                for stp in _merge(lists):
                    stp()
            S.flush(block)


TG = 16
JH = 64


def _phase2(nc, S, nb, G):
    from contextlib import ExitStack
    xn2T_v, h1T_v, pT_v, outT_v, ijg = G["xn2T_v"], G["h1T_v"], G["pT_v"], G["outT_v"], G["ijg"]
    UTb, VVb = G["UTb"], G["VVb"]
    UTb_t, VVb_t, h1T_t, xn2T_t, ijg_t = G["UTb_t"], G["VVb_t"], G["h1T_t"], G["xn2T_t"], G["ijg_t"]
    outT_t = _views(G["outT"], nb)
    with ExitStack() as st:
        def sb(name, shape, dt):
            return Tile(st.enter_context(nc.sbuf_tensor("b_" + name, shape, dt)), name)

        def psum(name):
            return Tile(st.enter_context(nc.psum_tensor("b_" + name, [128, 512], F32)), name)

        C = Ctx()
        w_pg_sb = sb("w_pg", [128, 8, 1024], BF16)
        w_pp_sb = sb("w_pp", [128, 2, 1024], BF16)
        small_sb = sb("small", [128, 48], F32)
        C.ones = sb("ones", [128, 128], BF16)
        C.epsb = sb("epsb", [128, 1], F32)
        iota_f = sb("iota_f", [128, 128], F32)
        iota3 = sb("iota3", [128, TG, 128], BF16)
        stg = [sb("stg%d" % i, [128, 1024], F32) for i in range(1)]
        Gh = [sb("G%d" % i, [128, JH, TB], BF16) for i in range(2)]
        NUB = 3
        ut = [sb("ut%d" % i, [128, 4096], BF16) for i in range(NUB)]
        vt = [sb("vt%d" % i, [128, 4096], BF16) for i in range(NUB)]
        xn2b = [sb("xn2b%d" % i, [128, 8, TB], BF16) for i in range(2)]
        h1b = [sb("h1b%d" % i, [128, 8, TB], F32) for i in range(2)]
        pTb = [sb("pTb%d" % i, [128, 2, TB], F32) for i in range(2)]
        pTbb = sb("pTbb", [128, 2, TB], BF16)
        ijgb = [sb("ijgb%d" % i, [128, 3, TB], BF16) for i in range(2)]
        OI = [sb("OI%d" % i, [128, TG, 128], BF16) for i in range(2)]
        OJ = [sb("OJ%d" % i, [128, TG, JH], BF16) for i in range(2)]
        sq = sb("sq", [128, 8, TB], BF16)
        rs = sb("rs", [128, TB], F32)
        xn3 = sb("xn3", [128, 8, TB], BF16)
        sg = [sb("sg%d" % i, [128, TB], F32) for i in range(2)]
        outb = sb("outb", [128, 8, TB], F32)
        acc = [psum("acc%d" % i) for i in range(4)]
        pa = [psum("pa%d" % i) for i in range(2)]
        pw = [psum("pw%d" % i) for i in range(2)]
        G_v = [_views(Gh[i], JH) for i in range(2)]
        Hd = [Tile(Gh[i].t, "Hd%d" % i) for i in range(2)]
        gple = Tile(small_sb.t[:, 16:24])
        gfin = Tile(small_sb.t[:, 24:32])

        with nc.Block() as block:
            S.dma("sp", lambda e: e.dma_start(out=small_sb[:], in_=G["small"][:]), reads=[G["small"]], writes=[small_sb])
            gple.lw = gfin.lw = small_sb.lw
            S.dma("sp", lambda e: e.dma_start(out=iota_f[:], in_=G["iota_d"][:, :]), reads=[G["iota_d"]], writes=[iota_f])
            S.op("pool", lambda e: e.memset(C.ones[:], 1.0), writes=[C.ones])
            S.op("pool", lambda e: e.memset(C.epsb[:], EPS), writes=[C.epsb])
            S.op("dve", lambda e: e.tensor_copy(out=iota3[:, :, :], in_=iota_f[:, :].unsqueeze(1).broadcast_to([128, TG, 128])),
                 reads=[iota_f], writes=[iota3])
            wl_thunks = []
            for c in range(8):
                wl_thunks.append(lambda c=c: _load_cast(S, stg, w_pg_sb, (slice(None), c, slice(None)), G["w_pg"],
                                                        (slice(c * 128, (c + 1) * 128), slice(None)), 1024, c))
            for c in range(2):
                wl_thunks.append(lambda c=c: _load_cast(S, stg, w_pp_sb, (slice(None), c, slice(None)), G["w_pp"],
                                                        (slice(c * 128, (c + 1) * 128), slice(None)), 1024, 8 + c))

            nload = [0, 0]

            def emit_loads(b):
                t0 = b * TB
                S.dma("sp", lambda e: e.dma_start(out=xn2b[b % 2][:, :, :], in_=xn2T_v[:, :, t0:t0 + TB]), reads=[xn2T_t[b]], writes=[xn2b[b % 2]])
                S.dma("sp", lambda e: e.dma_start(out=ijgb[b % 2][:, :, :], in_=ijg[:, :, t0:t0 + TB]), reads=[ijg_t[b]], writes=[ijgb[b % 2]])

            def emit_loads2(b):
                t0 = b * TB
                S.dma("sp", lambda e: e.dma_start(out=h1b[b % 2][:, :, :], in_=h1T_v[:, :, t0:t0 + TB]), reads=[h1T_t[b]], writes=[h1b[b % 2]])
                S.dma("sp", lambda e: e.dma_start(out=pTb[b % 2][:, :, :], in_=pT_v[:, :, t0:t0 + TB]), reads=[G["pT"]], writes=[pTb[b % 2]])

            prebuilt = set()

            def w_oct(kw, oc8):
                b, hf = kw // 2, kw % 2
                ij = ijgb[b % 2]
                gh = Gh[kw % 2]
                tg = oc8 // 2
                oi = OI[tg % 2]
                oj = OJ[tg % 2]
                def build(tgb):
                    oi_, oj_ = OI[tgb % 2], OJ[tgb % 2]
                    tk = slice(tgb * TG, (tgb + 1) * TG)
                    S.op("dve", lambda e: e.tensor_tensor(
                        out=oi_[:, :, :], in0=iota3[:, :, :], in1=ij[:, 0, tk].unsqueeze(2).broadcast_to([128, TG, 128]), op=ALU.is_equal),
                        reads=[iota3, ij], writes=[oi_])
                    S.op("dve", lambda e: e.tensor_tensor(
                        out=oj_[:, :, :], in0=iota3[:, :, hf * JH:(hf + 1) * JH], in1=ij[:, 1, tk].unsqueeze(2).broadcast_to([128, TG, JH]), op=ALU.is_equal),
                        reads=[iota3, ij], writes=[oj_])
                    S.op("pool", lambda e: e.tensor_tensor(
                        out=oj_[:, :, :], in0=oj_[:, :, :], in1=ij[:, 2, tk].unsqueeze(2).broadcast_to([128, TG, JH]), op=ALU.mult),
                        reads=[oj_, ij], writes=[oj_])
                if oc8 < 0:
                    build(0)
                    prebuilt.add(kw)
                    return
                if oc8 % 2 == 0:
                    if tg == 0 and kw not in prebuilt:
                        build(0)
                    if tg + 1 < TB // TG:
                        build(tg + 1)
                pp = pw[oc8 % 2]

                def mm(e, oi=oi, oj=oj, oc8=oc8, pp=pp):
                    ins = None
                    for tl in range(8):
                        tt = (oc8 % 2) * 8 + tl
                        ins = e.matmul(pp[:, :].rearrange("p (j t) -> p j t", t=8)[:, :, tl], lhsT=oi[:, tt, :], rhs=oj[:, tt, :],
                                       start=(tl == 0), stop=(tl == 7), skip_group_check=True)
                    return ins
                S.op("pe", mm, reads=[oi, oj], writes=[pp])
                tq = oc8 * 8
                S.op("dve", lambda e, pp=pp, tq=tq, gh=gh: e.tensor_tensor(
                    out=gh[:, :, tq:tq + 8],
                    in0=pp[:, :].rearrange("p (j t) -> p j t", t=8),
                    in1=gh[:, :, tq:tq + 8], op=ALU.mult),
                    reads=G_v[kw % 2] + [pp], writes=[Hd[kw % 2]], skip_same=True)

            def emit_A(ka, kw):
                b, hf = ka // 2, ka % 2
                if hf == 0:
                    emit_loads(b)
                else:
                    emit_loads2(b)
                xb = xn2b[b % 2]
                gh = Gh[ka % 2]
                oc8 = 0
                for gl in range(JH // 4):
                    gq = hf * (JH // 4) + gl
                    u = ut[nload[0] % NUB]
                    nload[0] += 1
                    S.dma("sp", lambda e, u=u, gq=gq: e.dma_start(out=u[:, :], in_=UTb[:, gq * 4096:(gq + 1) * 4096]),
                          reads=[UTb_t[gq]], writes=[u])
                    if wl_thunks and gl >= 2:
                        wl_thunks.pop(0)()
                    for jj in range(4):
                        jl = gl * 4 + jj
                        pp = pa[jl % 2]

                        def mm(e, u=u, jj=jj, pp=pp, xb=xb):
                            ins = None
                            for dc in range(8):
                                o = (jj * 8 + dc) * 128
                                ins = e.matmul(pp[:, 0:TB], lhsT=u[:, o:o + 128], rhs=xb[:, dc, :], start=(dc == 0), stop=(dc == 7))
                            return ins
                        S.op("pe", mm, reads=[u, xb], writes=[pp])
                        S.op("act", lambda e, jl=jl, pp=pp, gh=gh: e.activation(out=gh[:, jl, :], in_=pp[:, 0:TB], func=AF.Gelu_apprx_tanh),
                             reads=[pp], writes=[G_v[ka % 2][jl]])
                        if kw is not None and jl % 2 == 1:
                            w_oct(kw, oc8)
                            oc8 += 1

            def emit_W_only(kw):
                for oc8 in range(TB // 8):
                    w_oct(kw, oc8)

            def emit_V(kv, inter):
                b, hf = kv // 2, kv % 2
                gh = Gh[kv % 2]
                for gl in range(JH // 4):
                    if inter:
                        inter.pop(0)()
                    gq = hf * (JH // 4) + gl
                    v = vt[nload[1] % NUB]
                    nload[1] += 1
                    S.dma("sp", lambda e, v=v, gq=gq: e.dma_start(out=v[:, :], in_=VVb[:, gq * 4096:(gq + 1) * 4096]),
                          reads=[VVb_t[gq]], writes=[v])
                    for jj in range(4):
                        jl = gl * 4 + jj
                        j = hf * JH + jl

                        def mm(e, v=v, jj=jj, j=j, jl=jl, gh=gh):
                            ins = None
                            for dc in range(8):
                                o = jj * 1024 + dc * 128
                                ins = e.matmul(acc[dc // 2][:, (dc % 2) * TB:(dc % 2 + 1) * TB], lhsT=v[:, o:o + 128], rhs=gh[:, jl, :],
                                               start=(j == 0 and dc % 2 == 0), stop=(j == 127), skip_group_check=True)
                            return ins
                        S.op("pe", mm, reads=[v, G_v[kv % 2][jl], Hd[kv % 2]], writes=acc)

            def norm_pre(h):
                for c in range(8):
                    if c % 2 == 0:
                        S.op("act", lambda e, c=c: e.activation(out=sq[:, c, :], in_=h[:, c, :], func=AF.Square), reads=[h], writes=[sq])
                    else:
                        S.op("pool", lambda e, c=c: e.tensor_tensor(out=sq[:, c, :], in0=h[:, c, :], in1=h[:, c, :], op=ALU.mult),
                             reads=[h], writes=[sq])

            def stage_E1a(b):
                h = h1b[b % 2]
                for kk in range(4):
                    S.op("dve", lambda e, kk=kk: e.tensor_tensor(
                        out=h[:, 2 * kk:2 * kk + 2, :], in0=acc[kk][:, :].rearrange("p (c t) -> p c t", t=TB),
                        in1=h[:, 2 * kk:2 * kk + 2, :], op=ALU.add), reads=[acc[kk], h], writes=[h])
                S.op("pool", lambda e: e.tensor_copy(out=pTbb[:, :, :], in_=pTb[b % 2][:, :, :]), reads=[pTb[b % 2]], writes=[pTbb])
                norm_pre(h)

            def norm_post_a():
                def mm(e):
                    ins = None
                    for c in range(8):
                        ins = e.matmul(pa[0][:, 0:TB], lhsT=C.ones[:, :], rhs=sq[:, c, :], start=(c == 0), stop=(c == 7))
                    return ins
                S.op("pe", mm, reads=[sq, C.ones], writes=[pa[0]])
                S.op("act", lambda e: e.activation(out=rs[:, :], in_=pa[0][:, 0:TB], func=AF.Sqrt, bias=C.epsb[:, 0:1], scale=1.0 / D),
                     reads=[pa[0], C.epsb], writes=[rs])
                S.op("dve", lambda e: e.reciprocal(out=rs[:, :], in_=rs[:, :]), reads=[rs], writes=[rs])

            def norm_post_b(h, g_sb, dst):
                for c in range(8):
                    S.op("dve", lambda e, c=c: e.scalar_tensor_tensor(out=dst[:, c, :], in0=h[:, c, :], scalar=g_sb[:, c:c + 1], in1=rs[:, :],
                                                                      op0=ALU.mult, op1=ALU.mult), reads=[h, g_sb, rs], writes=[dst])

            def ple_oc(b, oc):
                h = h1b[b % 2]
                pp = (pa + pw)[oc % 4]
                sgt = sg[oc % 2]

                def mm(e):
                    for c in range(8):
                        e.matmul(pp[:, 0:TB], lhsT=w_pg_sb[:, c, oc * 128:(oc + 1) * 128], rhs=xn3[:, c, :], start=(c == 0), stop=(c == 7))
                    ins = None
                    for c in range(2):
                        ins = e.matmul(pp[:, TB:2 * TB], lhsT=w_pp_sb[:, c, oc * 128:(oc + 1) * 128], rhs=pTbb[:, c, :],
                                       start=(c == 0), stop=(c == 1))
                    return ins
                S.op("pe", mm, reads=[w_pg_sb, w_pp_sb, xn3, pTbb], writes=[pp])
                S.op("act", lambda e: e.activation(out=sgt[:, :], in_=pp[:, 0:TB], func=AF.Sigmoid), reads=[pp], writes=[sgt])
                S.op("dve", lambda e: e.tensor_tensor(out=sgt[:, :], in0=pp[:, TB:2 * TB], in1=sgt[:, :], op=ALU.mult), reads=[pp, sgt], writes=[sgt])
                S.op("dve", lambda e: e.tensor_tensor(out=h[:, oc, :], in0=h[:, oc, :], in1=sgt[:, :], op=ALU.add), reads=[h, sgt], writes=[h])

            toks = []

            def store_out(b):
                t0 = b * TB
                toks.append(S.dma("sp", lambda e: e.dma_start(out=outT_v[:, :, t0:t0 + TB], in_=outb[:, :, :]), reads=[outb], writes=[outT_t[b]]))

            def epilogue_thunks(b):
                h = h1b[b % 2]
                th = [norm_post_a, lambda: norm_post_b(h, gple, xn3)]
                for oc in range(8):
                    th.append(lambda oc=oc: ple_oc(b, oc))
                th.append(lambda: norm_pre(h))
                th.append(norm_post_a)
                th.append(lambda: norm_post_b(h, gfin, outb))
                th.append(lambda: store_out(b))
                return th

            pending = []

            nk = 2 * nb
            emit_A(0, None)
            for kk_ in range(1, nk + 1):
                if kk_ < nk:
                    emit_A(kk_, kk_ - 1)
                else:
                    emit_W_only(kk_ - 1)
                if kk_ < nk:
                    w_oct(kk_, -1)
                emit_V(kk_ - 1, pending)
                if (kk_ - 1) % 2 == 1:
                    bdone = (kk_ - 1) // 2
                    stage_E1a(bdone)
                    pending.extend(epilogue_thunks(bdone))
            while pending:
                pending.pop(0)()
            S.wait_all("sp", toks)
            S.flush(block)


def _prep_shared(inp, nb):
    f = np.float32
    sh = {}
    sh["w_in"] = np.ascontiguousarray(inp["w_in"][0], dtype=f)
    sh["w_o"] = np.ascontiguousarray(inp["w_o"][0], dtype=f)
    sh["w_q"] = np.ascontiguousarray(inp["w_q"][0], dtype=f)
    sh["w_pg"] = np.ascontiguousarray(inp["w_ple_gate"][0], dtype=f)
    sh["w_pp"] = np.ascontiguousarray(inp["w_ple_proj"][0], dtype=f)
    sh["keysT"] = np.ascontiguousarray(np.asarray(inp["sub_keys"][0]).reshape(16, 128, 128).transpose(2, 0, 1).reshape(128, 2048), dtype=f)
    sh["poolw"] = np.ascontiguousarray(np.asarray(inp["pool_w"][0]).transpose(1, 0, 2).reshape(128, 512), dtype=f)
    small = np.zeros((128, 48), dtype=f)
    small[:, 0:8] = np.asarray(inp["g_mix"][0]).reshape(8, 128).T
    small[:, 8:16] = np.asarray(inp["g_ffn"][0]).reshape(8, 128).T
    small[:, 16:24] = np.asarray(inp["g_ple"][0]).reshape(8, 128).T
    small[:, 24:32] = np.asarray(inp["g_final"]).reshape(8, 128).T
    small[:, 32:44] = np.asarray(inp["conv_w"][0]).T.reshape(4, 128, 3).transpose(1, 0, 2).reshape(128, 12)
    small[:, 44:48] = np.asarray(inp["pool_scale"][0]).reshape(4, 128).T
    sh["small"] = small
    sh["UT"] = np.ascontiguousarray(np.asarray(inp["expert_u"][0]).reshape(128, 128, 8, 128).transpose(3, 1, 2, 0).reshape(128, 131072), dtype=f)
    sh["VV"] = np.ascontiguousarray(np.asarray(inp["expert_v"][0]).reshape(128, 131072), dtype=f)
    sh["ident"] = np.eye(128, dtype=f)
    sh["iota"] = np.tile(np.arange(128, dtype=f), (128, 1))
    bits = np.zeros((128, 256), dtype=np.uint32)
    bits[:, 0:128] = np.uint32(0xFFFFFF80)
    bits[:, 128:256] = np.arange(128, dtype=np.uint32)[None, :]
    sh["bits"] = bits
    seq = nb * TB
    edge = np.ones((2, 4, 8), dtype=f)
    for g in range(4):
        w = 2 << g
        for c in range(8):
            for a, t in ((0, c), (1, seq - 8 + c)):
                lo = min(max(t - w // 2, 0), seq)
                hi = min(max(t + w // 2, 0), seq)
                edge[a, g, c] = w / float(hi - lo)
    sh["edge"] = np.tile(edge.reshape(1, 64), (128, 1)).astype(f)
    return sh


def _prep_core(inp, sh, b, nb):
    seq = nb * TB
    m = dict(sh)
    m["xT"] = np.ascontiguousarray(np.asarray(inp["x"][b, :seq]).T, dtype=np.float32)
    m["pT"] = np.ascontiguousarray(np.asarray(inp["p"][0, b, :seq]).T, dtype=np.float32)
    return m


def kernel(**inputs):
    nb = SEQ // TB
    sh = _prep_shared(inputs, nb)
    in_maps = [_prep_core(inputs, sh, b, nb) for b in range(NCORES)]
    nc = bass.Bass("TRN2", target_bir_lowering=False)
    build_program(nc, nb, debug=False)
    res = run_bass_kernel_spmd(nc, in_maps, core_ids=list(range(NCORES)))
    out = np.stack([np.ascontiguousarray(np.asarray(r["outT"]).T) for r in res.results], axis=0)
    return out.astype(np.float32)
```

```python
import numpy as np
import ml_dtypes
import concourse.bass as bass
import concourse.mybir as mybir
from concourse.bass_utils import run_bass_kernel_spmd

F32 = mybir.dt.float32
BF16 = mybir.dt.bfloat16
U32 = mybir.dt.uint32
I32 = mybir.dt.int32
ALU = mybir.AluOpType
AF = mybir.ActivationFunctionType
AX = mybir.AxisListType

D = 1024
SEQ = 4096
NCORES = 8
TB = 256
HALO = 8
TW = TB + 2 * HALO
EPS = 1e-6
NEG = -3.0e38

DMA_SLOTS = {"sp": 8, "act": 4, "pool": 4}
STRICT_WAR = True


class Tile:
    __slots__ = ("t", "lw", "rd", "name")

    def __init__(self, t, name=""):
        self.t = t
        self.lw = None
        self.rd = []
        self.name = name

    def __getitem__(self, k):
        return self.t[k]


class Sched:
    def __init__(self, nc, sems):
        self.nc = nc
        self.sems = sems
        self.q = {e: [] for e in ("sp", "act", "dve", "pool", "pe")}
        self.cnt = {e: 0 for e in self.q}
        self.ndma = {e: 0 for e in DMA_SLOTS}
        self.seen = {e: {} for e in self.q}

    def _deps(self, eng, reads, writes):
        deps = []
        for t in reads:
            if t.lw is not None:
                deps.append(t.lw)
        for t in writes:
            if t.lw is not None:
                deps.append(t.lw)
            for r in t.rd:
                if r[0] == "c" and r[1] == eng and not STRICT_WAR:
                    continue
                deps.append(r)
        return deps

    def _waits(self, eng, deps):
        w = {}
        for d in deps:
            if d[0] == "c":
                _, y, n = d
                if y == eng and eng == "pe":
                    continue
                key = ("c", y)
                val = n
            else:
                _, qn, n = d
                k = DMA_SLOTS[qn]
                key = ("d", qn, n % k)
                val = 16 * (n // k + 1)
            if self.seen[eng].get(key, 0) >= val:
                continue
            if w.get(key, 0) < val:
                w[key] = val
        for key, v in w.items():
            self.seen[eng][key] = v
        return [(self.sems[key], v) for key, v in w.items()]

    def _mark(self, tok, reads, writes):
        for t in reads:
            t.rd.append(tok)
        for t in writes:
            t.lw = tok
            t.rd = []

    def op(self, eng, fn, reads=(), writes=(), skip_same=False):
        deps = self._deps(eng, reads, writes)
        if skip_same:
            deps = [d for d in deps if not (d[0] == "c" and d[1] == eng)]
        waits = self._waits(eng, deps)
        self.cnt[eng] += 1
        tok = ("c", eng, self.cnt[eng])
        self._mark(tok, reads, writes)
        self.q[eng].append((waits, fn, (self.sems[("c", eng)], 1)))
        return tok

    def dma(self, qn, fn, reads=(), writes=()):
        n = self.ndma[qn]
        k = DMA_SLOTS[qn]
        deps = self._deps(qn, reads, writes)
        if n >= k:
            deps.append(("d", qn, n - k))
        waits = self._waits(qn, deps)
        self.ndma[qn] += 1
        tok = ("d", qn, n)
        self._mark(tok, reads, writes)
        self.q[qn].append((waits, fn, (self.sems[("d", qn, n % k)], 16)))
        return tok

    def wait_all(self, eng, toks):
        waits = self._waits(eng, list(toks))
        if waits:
            self.q[eng].append((waits, None, None))

    def drain(self):
        toks = []
        for qn, k in DMA_SLOTS.items():
            n = self.ndma[qn]
            toks.extend(("d", qn, i) for i in range(max(0, n - k), n))
        self.wait_all("sp", toks)

    def flush(self, block):
        self.drain()
        for name, method in (("sp", block.sync), ("act", block.scalar), ("dve", block.vector),
                             ("pool", block.gpsimd), ("pe", block.tensor)):
            ops = self.q[name]
            self.q[name] = []
            if not ops:
                continue

            def body(e, ops=ops):
                for waits, fn, inc in ops:
                    for sem, val in waits:
                        e.wait_ge(sem, val)
                    if fn is None:
                        continue
                    ins = fn(e)
                    ins.then_inc(inc[0], inc[1])
            method(body)


def _alloc_sems(nc, stack):
    sems = {}
    for e in ("act", "dve", "pool", "pe"):
        sems[("c", e)] = stack.enter_context(nc.semaphore("c_" + e))
    for qn, k in DMA_SLOTS.items():
        for i in range(k):
            sems[("d", qn, i)] = stack.enter_context(nc.semaphore("d_%s%d" % (qn, i)))
    return sems


CAST_W = 4096


def _cast_block(nc, S, src, dst, nelem, tag):
    from contextlib import ExitStack
    npieces = nelem // CAST_W
    with ExitStack() as st:
        NBUF = 3
        fin = [Tile(st.enter_context(nc.sbuf_tensor("cin%s%d" % (tag, i), [128, CAST_W], F32))) for i in range(NBUF)]
        fout = [Tile(st.enter_context(nc.sbuf_tensor("cout%s%d" % (tag, i), [128, CAST_W], BF16))) for i in range(NBUF)]
        with nc.Block() as block:
            for pc in range(npieces):
                b = pc % NBUF
                sl = slice(pc * CAST_W, (pc + 1) * CAST_W)
                S.dma("sp", lambda e, b=b, sl=sl: e.dma_start(out=fin[b][:], in_=src[:, sl]),
                      reads=[src], writes=[fin[b]])
                eng = ("dve", "act", "pool")[pc % 3] if False else ("dve", "act")[pc % 2]
                if eng == "act":
                    S.op("act", lambda e, b=b: e.copy(out=fout[b][:], in_=fin[b][:]), reads=[fin[b]], writes=[fout[b]])
                else:
                    S.op(eng, lambda e, b=b: e.tensor_copy(out=fout[b][:], in_=fin[b][:]), reads=[fin[b]], writes=[fout[b]])
                S.dma("act", lambda e, b=b, sl=sl: e.dma_start(out=dst[:, sl], in_=fout[b][:]),
                      reads=[fout[b]], writes=[dst])
            S.flush(block)


def _views(tile_, n):
    return [Tile(tile_.t if isinstance(tile_, Tile) else tile_) for _ in range(n)]


class Ctx:
    pass


def _rmsnorm(nc, S, C, src, g_sb, dst, width, ps, sq, rs, tagw=None):
    for c in range(8):
        eng = "act" if c % 2 == 0 else "pool"
        if eng == "act":
            S.op("act", lambda e, c=c: e.activation(out=sq[:, c, 0:width], in_=src[:, c, 0:width], func=AF.Square),
                 reads=[src], writes=[sq])
        else:
            S.op("pool", lambda e, c=c: e.tensor_tensor(out=sq[:, c, 0:width], in0=src[:, c, 0:width],
                                                        in1=src[:, c, 0:width], op=ALU.mult),
                 reads=[src], writes=[sq])

    def mm(e):
        ins = None
        for c in range(8):
            ins = e.matmul(ps[:, 0:width], lhsT=C.ones[:, :], rhs=sq[:, c, 0:width], start=(c == 0), stop=(c == 7))
        return ins
    S.op("pe", mm, reads=[sq, C.ones], writes=[ps])
    S.op("act", lambda e: e.activation(out=rs[:, 0:width], in_=ps[:, 0:width], func=AF.Sqrt,
                                       bias=C.epsb[:, 0:1], scale=1.0 / D),
         reads=[ps, C.epsb], writes=[rs])
    S.op("dve", lambda e: e.reciprocal(out=rs[:, 0:width], in_=rs[:, 0:width]), reads=[rs], writes=[rs])
    for c in range(8):
        S.op("dve", lambda e, c=c: e.scalar_tensor_tensor(out=dst[:, c, 0:width], in0=src[:, c, 0:width],
                                                          scalar=g_sb[:, c:c + 1], in1=rs[:, 0:width],
                                                          op0=ALU.mult, op1=ALU.mult),
             reads=[src, g_sb, rs], writes=[dst])


def _load_cast(S, stg, dst, dst_sl, src, src_sl, width, i):
    st_ = stg[i % len(stg)]
    S.dma("sp", lambda e: e.dma_start(out=st_[:, 0:width], in_=src[src_sl]), reads=[src], writes=[st_])
    if i % 2 == 0:
        S.op("dve", lambda e: e.tensor_copy(out=dst[dst_sl], in_=st_[:, 0:width]), reads=[st_], writes=[dst])
    else:
        S.op("act", lambda e: e.copy(out=dst[dst_sl], in_=st_[:, 0:width]), reads=[st_], writes=[dst])


def build_program(nc, nb, debug=False):
    from contextlib import ExitStack
    seq = nb * TB
    okind = "ExternalOutput" if debug else "Internal"

    def din(name, shape, dt=F32):
        return Tile(nc.dram_tensor(name, shape, dt, kind="ExternalInput").ap(), name)

    xT = din("xT", [D, seq])
    pT = din("pT", [256, seq])
    w_in = din("w_in", [D, 2048])
    w_o = din("w_o", [D, D])
    w_q = din("w_q", [D, 2048])
    w_pg = din("w_pg", [D, D])
    w_pp = din("w_pp", [256, D])
    keysT = din("keysT", [128, 2048])
    poolw = din("poolw", [128, 512])
    small = din("small", [128, 48])
    UT = din("UT", [128, 131072])
    VV = din("VV", [128, 131072])
    ident_d = din("ident", [128, 128])
    edge_d = din("edge", [128, 64])
    iota_d = din("iota", [128, 128])
    bits_d = din("bits", [128, 256], U32)
    outT = Tile(nc.dram_tensor("outT", [D, seq], F32, kind="ExternalOutput").ap(), "outT")
    UTb = nc.dram_tensor("UTb", [128, 131072], BF16, kind="Internal").ap()
    VVb = nc.dram_tensor("VVb", [128, 131072], BF16, kind="Internal").ap()
    h1T = nc.dram_tensor("h1T", [D, seq], F32, kind=okind).ap()
    xn2T = nc.dram_tensor("xn2T", [D, seq], BF16, kind=okind).ap()
    ijg = nc.dram_tensor("ijg", [128, 3, seq], BF16, kind=okind).ap()
    NPIECE = 131072 // CAST_W
    UTb_t = _views(UTb, NPIECE)
    VVb_t = _views(VVb, NPIECE)
    h1T_t = _views(h1T, nb)
    xn2T_t = _views(xn2T, nb)
    ijg_t = _views(ijg, nb)

    xT_v = xT.t.rearrange("(c p) t -> p c t", p=128)
    pT_v = pT.t.rearrange("(c p) t -> p c t", p=128)
    outT_v = outT.t.rearrange("(c p) t -> p c t", p=128)
    h1T_v = h1T.rearrange("(c p) t -> p c t", p=128)
    xn2T_v = xn2T.rearrange("(c p) t -> p c t", p=128)

    with ExitStack() as top:
        sems = _alloc_sems(nc, top)
        S = Sched(nc, sems)


        _phase1(nc, S, nb, locals())

        _phase2(nc, S, nb, locals())
    return nc


def _cast_tables(nc, S, UT, UTb_t, VV, VVb_t):
    from contextlib import ExitStack
    with ExitStack() as st:
        NBUF = 3
        fin = [Tile(st.enter_context(nc.sbuf_tensor("cin%d" % i, [128, CAST_W], F32))) for i in range(NBUF)]
        fout = [Tile(st.enter_context(nc.sbuf_tensor("cout%d" % i, [128, CAST_W], BF16))) for i in range(NBUF)]
        with nc.Block() as block:
            k = 0
            for src, dst_t in ((UT, UTb_t), (VV, VVb_t)):
                for pc in range(len(dst_t)):
                    b = k % NBUF
                    sl = slice(pc * CAST_W, (pc + 1) * CAST_W)
                    S.dma("sp", lambda e, b=b, sl=sl, src=src: e.dma_start(out=fin[b][:], in_=src[:, sl]),
                          reads=[src], writes=[fin[b]])
                    eng = ("dve", "act", "pool")[k % 3]
                    if eng == "act":
                        S.op("act", lambda e, b=b: e.copy(out=fout[b][:], in_=fin[b][:]),
                             reads=[fin[b]], writes=[fout[b]])
                    else:
                        S.op(eng, lambda e, b=b: e.tensor_copy(out=fout[b][:], in_=fin[b][:]),
                             reads=[fin[b]], writes=[fout[b]])
                    d = dst_t[pc]
                    S.dma("act", lambda e, b=b, sl=sl, d=d: e.dma_start(out=d[:, sl], in_=fout[b][:]),
                          reads=[fout[b]], writes=[d])
                    k += 1
            S.flush(block)


def _nop():
    pass


def _merge(lists):
    lists = [l for l in lists if l]
    idx = [0] * len(lists)
    out = []
    total = sum(len(l) for l in lists)
    for _ in range(total):
        best = None
        for i, l in enumerate(lists):
            if idx[i] < len(l):
                frac = (idx[i] + 0.5) / len(l)
                if best is None or frac < best[0]:
                    best = (frac, i)
        i = best[1]
        out.append(lists[i][idx[i]])
        idx[i] += 1
    return out


def _phase1(nc, S, nb, G):
    from contextlib import ExitStack
    xT_v, h1T_v, xn2T_v, ijg = G["xT_v"], G["h1T_v"], G["xn2T_v"], G["ijg"]
    xT, h1T_t, xn2T_t, ijg_t = G["xT"], G["h1T_t"], G["xn2T_t"], G["ijg_t"]
    seq = nb * TB
    CW2 = 2048
    with ExitStack() as st:
        def sb(name, shape, dt):
            return Tile(st.enter_context(nc.sbuf_tensor("a_" + name, shape, dt)), name)

        def psum(name):
            return Tile(st.enter_context(nc.psum_tensor("a_" + name, [128, 512], F32)), name)

        C = Ctx()
        w_in_sb = sb("w_in", [128, 8, 2048], BF16)
        w_o_sb = sb("w_o", [128, 8, 1024], BF16)
        w_q_sb = sb("w_q", [128, 8, 2048], BF16)
        keys_sb = sb("keys", [128, 16, 128], BF16)
        poolw_sb = sb("poolw", [128, 4, 128], BF16)
        small_sb = sb("small", [128, 48], F32)
        ident = sb("ident", [128, 128], F32)
        C.ones = sb("ones", [128, 128], BF16)
        C.epsb = sb("epsb", [128, 1], F32)
        iota16 = sb("iota16", [128, 16], F32)
        bits_sb = sb("bits", [128, 256], U32)
        edge = sb("edge", [128, 2, 4, 8], F32)
        stg = [sb("stg%d" % i, [128, CW2], F32) for i in range(2)]
        cout = [sb("cout%d" % i, [128, CW2], BF16) for i in range(2)]

        xt = sb("xt", [128, 8, TW], F32)
        sq = sb("sq", [128, 8, TW], BF16)
        rs = sb("rs", [128, TW], F32)
        xn = sb("xn", [128, 8, TW], BF16)
        bsb = sb("bsb", [128, 4, TB], BF16)
        z = sb("z", [128, 4, TW], F32)
        usb = sb("usb", [128, 4, TW], F32)
        pa_ = sb("pa", [128, TW], F32)
        pb_ = sb("pb", [128, TW], F32)
        tcv = sb("tcv", [128, 4, TB], F32)
        tcv_v = _views(tcv, 4)
        pooled = sb("pooled", [128, 4, TB], BF16)
        xn2_ = [sb("xn2_%d" % i, [128, 8, TB], BF16) for i in range(2)]
        qT = [sb("qT%d" % i, [128, 16, TB], BF16) for i in range(2)]
        scrA = sb("scrA", [128, 2048], F32)
        cand = sb("cand", [128, 8, 256], F32)
        sv = sb("sv", [128, 16, 16], F32)
        si = sb("si", [128, 16, 16], U32)
        sif = sb("sif", [128, 16, 16], F32)
        top = sb("top", [128, 8, 16], F32)
        pos = sb("pos", [128, 8, 16], U32)
        ex = sb("ex", [128, 8, 16], F32)
        zs = sb("zs", [128, 8], F32)
        gg = sb("gg", [128, 8, 16], F32)
        au = sb("au", [128, 8, 16], U32)
        bu = sb("bu", [128, 8, 16], U32)
        af = sb("af", [128, 8, 16], F32)
        bf = sb("bf", [128, 8, 16], F32)
        IJ = sb("IJ", [128, 2, 128], F32)
        ijg_sb = sb("ijg_sb", [128, 3, TB], BF16)
        P = [psum("p%d" % i) for i in range(8)]

        sA_v = _views(scrA, 8)
        sB_v = _views(scrA, 8)
        scrA_all = sA_v + sB_v
        sv_v = _views(sv, 16)
        si_v = _views(si, 16)
        cand_v = _views(cand, 8)
        top_v = _views(top, 8)
        pos_v = _views(pos, 8)
        s_sb = scrA.t[:, 0:1024].rearrange("p (q k) -> p q k", k=128)
        s_u = scrA.t[:, 0:1024].bitcast(U32).rearrange("p (q k) -> p q k", k=128)
        s2 = scrA.t[:, 1024:2048].rearrange("p (q k) -> p q k", k=128)
        oh = scrA.t[:, :].rearrange("p (s a) -> p s a", a=16)
        ycat = xn.t[:, :, 0:TB]
        h1 = xt.t[:, :, HALO:HALO + TB]

        gmix = Tile(small_sb.t[:, 0:8])
        gffn = Tile(small_sb.t[:, 8:16])
        convw = small_sb.t[:, 32:44].rearrange("p (k t) -> p k t", t=3)
        pscale = small_sb.t[:, 44:48]

        pj_i = [0]

        def pj():
            pj_i[0] += 1
            return (P[1], P[2], P[3], P[7])[pj_i[0] % 4]

        def norm_steps(src_t, src_ap, g_sb, dst_t, dst_ap, width):
            steps = []

            def s1():
                for c in range(8):
                    S.op("act", lambda e, c=c: e.activation(out=sq[:, c, 0:width], in_=src_ap(c), func=AF.Square), reads=[src_t], writes=[sq])
            steps.append(s1)
            steps.extend([_nop] * 2)

            def s2_():
                def mm(e):
                    ins = None
                    for c in range(8):
                        ins = e.matmul(P[0][:, 0:width], lhsT=C.ones[:, :], rhs=sq[:, c, 0:width], start=(c == 0), stop=(c == 7))
                    return ins
                S.op("pe", mm, reads=[sq, C.ones], writes=[P[0]])
                S.op("act", lambda e: e.activation(out=rs[:, 0:width], in_=P[0][:, 0:width], func=AF.Sqrt, bias=C.epsb[:, 0:1], scale=1.0 / D),
                     reads=[P[0], C.epsb], writes=[rs])
                S.op("dve", lambda e: e.reciprocal(out=rs[:, 0:width], in_=rs[:, 0:width]), reads=[rs], writes=[rs])
            steps.append(s2_)
            steps.extend([_nop] * 3)

            def s3():
                for c in range(8):
                    S.op("dve", lambda e, c=c: e.scalar_tensor_tensor(out=dst_ap(c), in0=src_ap(c), scalar=g_sb[:, c:c + 1], in1=rs[:, 0:width],
                                                                      op0=ALU.mult, op1=ALU.mult), reads=[src_t, g_sb, rs], writes=[dst_t])
            steps.append(s3)
            return steps

        def mixer_steps(b):
            steps = []
            t0 = b * TB
            lo = max(t0 - HALO, 0)
            hi = min(t0 + TB + HALO, seq)
            c_lo = lo - (t0 - HALO)
            c_hi = hi - (t0 - HALO)
            xn2 = xn2_[b % 2]

            def load():
                if c_lo > 0:
                    S.op("pool", lambda e: e.memset(xt[:, :, 0:HALO], 0.0), writes=[xt])
                if c_hi < TW:
                    S.op("pool", lambda e: e.memset(xt[:, :, TW - HALO:TW], 0.0), writes=[xt])
                S.dma("sp", lambda e: e.dma_start(out=xt[:, :, c_lo:c_hi], in_=xT_v[:, :, lo:hi]), reads=[xT], writes=[xt])
            steps.append(load)
            steps.extend(norm_steps(xt, lambda c: xt[:, c, 0:TW], gmix, xn, lambda c: xn[:, c, 0:TW], TW))

            def inproj(oc):
                pp = pj()

                def mm(e):
                    ins = None
                    for c in range(8):
                        ins = e.matmul(pp[:, 0:TW], lhsT=w_in_sb[:, c, oc * 128:(oc + 1) * 128], rhs=xn[:, c, :], start=(c == 0), stop=(c == 7))
                    return ins
                S.op("pe", mm, reads=[w_in_sb, xn], writes=[pp])
                kk = oc % 4
                if oc < 4:
                    S.op("act", lambda e: e.copy(out=bsb[:, kk, :], in_=pp[:, HALO:HALO + TB]), reads=[pp], writes=[bsb])
                elif oc < 8:
                    S.op("act", lambda e: e.copy(out=z[:, kk, :], in_=pp[:, 0:TW]), reads=[pp], writes=[z])
                elif oc < 12:
                    S.op("dve", lambda e: e.tensor_tensor(out=z[:, kk, :], in0=pp[:, 0:TW], in1=z[:, kk, :], op=ALU.mult), reads=[pp, z], writes=[z])
                else:
                    S.op("act", lambda e: e.copy(out=usb[:, kk, :], in_=pp[:, 0:TW]), reads=[pp], writes=[usb])
            ip_order = [12, 13, 14, 15, 4, 5, 6, 7, 8, 9, 10, 11, 0, 1, 2, 3]
            for n_, oc in enumerate(ip_order):
                steps.append(lambda oc=oc: inproj(oc))
                if 4 <= n_ < 8:
                    steps.append(lambda g=n_ - 4: pool(g))

            def conv(step):
                for kk in range(4):
                    tv = tcv_v[kk]
                    if step == 0:
                        S.op("dve", lambda e, kk=kk: e.tensor_scalar(out=tcv[:, kk, :], in0=z[:, kk, HALO:HALO + TB], scalar1=convw[:, kk, 1:2],
                                                                     scalar2=None, op0=ALU.mult), reads=[z, small_sb], writes=[tv])
                    elif step == 1:
                        S.op("dve", lambda e, kk=kk: e.scalar_tensor_tensor(out=tcv[:, kk, :], in0=z[:, kk, HALO - 1:HALO - 1 + TB], scalar=convw[:, kk, 0:1],
                                                                            in1=tcv[:, kk, :], op0=ALU.mult, op1=ALU.add), reads=[z, tv, small_sb], writes=[tv])
                    elif step == 2:
                        S.op("dve", lambda e, kk=kk: e.scalar_tensor_tensor(out=tcv[:, kk, :], in0=z[:, kk, HALO + 1:HALO + 1 + TB], scalar=convw[:, kk, 2:3],
                                                                            in1=tcv[:, kk, :], op0=ALU.mult, op1=ALU.add), reads=[z, tv, small_sb], writes=[tv])
                    else:
                        S.op("dve", lambda e, kk=kk: e.tensor_tensor(out=ycat[:, kk, :], in0=tcv[:, kk, :], in1=bsb[:, kk, :], op=ALU.mult),
                             reads=[tv, bsb], writes=[xn])
            for st_ in range(4):
                steps.append(lambda st_=st_: conv(st_))

            def pool(g):
                w = 2 << g
                cur = (usb, lambda sl: usb[:, g, sl])
                width = TW
                bufs = [pa_, pb_]
                for si_, step in enumerate((1, 2, 4, 8)[:g + 1]):
                    nxt_t = bufs[si_ % 2]
                    width2 = width - step
                    ct, cf = cur
                    S.op("pool", lambda e, cf=cf, nxt_t=nxt_t, width2=width2, step=step: e.tensor_tensor(
                        out=nxt_t[:, 0:width2], in0=cf(slice(0, width2)), in1=cf(slice(step, step + width2)), op=ALU.add),
                        reads=[ct], writes=[nxt_t])
                    cur = (nxt_t, lambda sl, nxt_t=nxt_t: nxt_t[:, sl])
                    width = width2
                ct, cf = cur
                off = HALO - w // 2
                edges = []
                if b == 0:
                    edges.append((0, 0))
                if b == nb - 1:
                    edges.append((1, TB - 8))
                for (ei, c0) in edges:
                    S.op("pool", lambda e, ei=ei, c0=c0: e.tensor_tensor(
                        out=cf(slice(off + c0, off + c0 + 8)), in0=cf(slice(off + c0, off + c0 + 8)), in1=edge[:, ei, g, :], op=ALU.mult),
                        reads=[ct, edge], writes=[ct])
                S.op("dve", lambda e: e.scalar_tensor_tensor(
                    out=pooled[:, g, :], in0=cf(slice(off, off + TB)), scalar=1.0 / w, in1=usb[:, g, HALO:HALO + TB],
                    op0=ALU.mult, op1=ALU.subtract), reads=[ct, usb], writes=[pooled])

            def pool_back(g):
                pp = pj()
                S.op("pe", lambda e: e.matmul(pp[:, 0:TB], lhsT=poolw_sb[:, g, :], rhs=pooled[:, g, :], start=True, stop=True),
                     reads=[poolw_sb, pooled], writes=[pp])
                S.op("act", lambda e: e.activation(out=ycat[:, 4 + g, :], in_=pp[:, 0:TB], func=AF.Copy, scale=pscale[:, g:g + 1]),
                     reads=[pp, small_sb], writes=[xn])
            for g in range(4):
                steps.append(lambda g=g: pool_back(g))

            def outproj(oc):
                pp = pj()

                def mm(e):
                    ins = None
                    for c in range(8):
                        ins = e.matmul(pp[:, 0:TB], lhsT=w_o_sb[:, c, oc * 128:(oc + 1) * 128], rhs=ycat[:, c, :], start=(c == 0), stop=(c == 7))
                    return ins
                S.op("pe", mm, reads=[w_o_sb, xn], writes=[pp])
                S.op("dve", lambda e: e.tensor_tensor(out=h1[:, oc, :], in0=pp[:, 0:TB], in1=h1[:, oc, :], op=ALU.add),
                     reads=[pp, xt], writes=[xt])
                S.op("act", lambda e: e.activation(out=sq[:, oc, 0:TB], in_=h1[:, oc, :], func=AF.Square), reads=[xt], writes=[sq])
            for oc in range(8):
                steps.append(lambda oc=oc: outproj(oc))
            steps.append(lambda: S.dma("sp", lambda e: e.dma_start(out=h1T_v[:, :, t0:t0 + TB], in_=h1), reads=[xt], writes=[h1T_t[b]]))
            steps.extend(norm_steps(xt, lambda c: h1[:, c, :], gffn, xn2, lambda c: xn2[:, c, :], TB)[3:])
            steps.append(lambda: S.dma("sp", lambda e: e.dma_start(out=xn2T_v[:, :, t0:t0 + TB], in_=xn2[:, :, :]), reads=[xn2], writes=[xn2T_t[b]]))

            return steps

        def q_steps(b):
            steps = []
            xn2 = xn2_[b % 2]
            qTb = qT[b % 2]

            def qproj(qc):
                pp = pj()

                def mm(e):
                    ins = None
                    for c in range(8):
                        ins = e.matmul(pp[:, 0:TB], lhsT=w_q_sb[:, c, qc * 128:(qc + 1) * 128], rhs=xn2[:, c, :], start=(c == 0), stop=(c == 7))
                    return ins
                S.op("pe", mm, reads=[w_q_sb, xn2], writes=[pp])
                S.op("act", lambda e: e.copy(out=qTb[:, qc, :], in_=pp[:, 0:TB]), reads=[pp], writes=[qTb])
            for qc in range(16):
                steps.append(lambda qc=qc: qproj(qc))
            return steps

        def routing_steps(b):
            steps = []
            t0 = b * TB
            qTb = qT[b % 2]

            def scores(tsl, half):
                for q4 in range(2):
                    pp = P[4 + q4]

                    def mm(e, pp=pp, q4=q4):
                        ins = None
                        for i in range(4):
                            qc = half * 8 + q4 * 4 + i
                            ins = e.matmul(pp[:, i * 128:(i + 1) * 128], lhsT=qTb[:, qc, tsl], rhs=keys_sb[:, qc, :], start=True, stop=True)
                        return ins
                    S.op("pe", mm, reads=[qTb, keys_sb], writes=[pp])
                    S.op("act", lambda e, pp=pp, q4=q4: e.copy(out=s_sb[:, q4 * 4:(q4 + 1) * 4, :], in_=pp[:, :].rearrange("p (q k) -> p q k", k=128)),
                         reads=[pp], writes=sA_v[q4 * 4:(q4 + 1) * 4])
                S.op("dve", lambda e: e.tensor_tensor(out=s_u, in0=s_u, in1=bits_sb[:, 0:128].unsqueeze(1).broadcast_to([128, 8, 128]),
                                                      op=ALU.bitwise_and), reads=sA_v + [bits_sb], writes=sA_v)
                S.op("dve", lambda e: e.tensor_tensor(out=s_u, in0=s_u, in1=bits_sb[:, 128:256].unsqueeze(1).broadcast_to([128, 8, 128]),
                                                      op=ALU.bitwise_or), reads=sA_v + [bits_sb], writes=sA_v)

            def topk_step(half, step):
                for q in range(8):
                    qc = half * 8 + q
                    if step == 0:
                        S.op("dve", lambda e, q=q, qc=qc: e.max(out=sv[:, qc, 0:8], in_=s_sb[:, q, :]), reads=[sA_v[q]], writes=[sv_v[qc]])
                    elif step == 1:
                        S.op("dve", lambda e, q=q, qc=qc: e.match_replace(out=s2[:, q, :], in_to_replace=sv[:, qc, 0:8], in_values=s_sb[:, q, :], imm_value=NEG),
                             reads=[sA_v[q], sv_v[qc]], writes=[sB_v[q]])
                    elif step == 2:
                        S.op("dve", lambda e, q=q, qc=qc: e.max(out=sv[:, qc, 8:16], in_=s2[:, q, :]), reads=[sB_v[q]], writes=[sv_v[qc]])
                    elif step == 3:
                        S.op("dve", lambda e, q=q, qc=qc: e.max_index(out=si[:, qc, 0:8], in_max=sv[:, qc, 0:8], in_values=s_sb[:, q, :]),
                             reads=[sA_v[q], sv_v[qc]], writes=[si_v[qc]])
                    else:
                        S.op("dve", lambda e, q=q, qc=qc: e.max_index(out=si[:, qc, 8:16], in_max=sv[:, qc, 8:16], in_values=s_sb[:, q, :]),
                             reads=[sA_v[q], sv_v[qc]], writes=[si_v[qc]])

            def idx_step():
                S.op("dve", lambda e: e.tensor_single_scalar(out=si[:, :, :], in_=sv.t[:, :, :].bitcast(U32), scalar=127, op=ALU.bitwise_and),
                     reads=sv_v, writes=si_v)

            def cand_step():
                sv4 = sv.t[:, :, :].rearrange("p (h two) k -> p h two k", two=2)
                S.op("dve", lambda e: e.tensor_tensor(
                    out=cand[:, :, :].rearrange("p h (a b) -> p h a b", b=16),
                    in0=sv4[:, :, 0, :].unsqueeze(3).broadcast_to([128, 8, 16, 16]),
                    in1=sv4[:, :, 1, :].unsqueeze(2).broadcast_to([128, 8, 16, 16]), op=ALU.add), reads=sv_v, writes=cand_v)

            def topk2_step(step):
                for h in range(8):
                    if step == 0:
                        S.op("dve", lambda e, h=h: e.max(out=top[:, h, 0:8], in_=cand[:, h, :]), reads=[cand_v[h]], writes=[top_v[h]])
                    elif step == 1:
                        S.op("dve", lambda e, h=h: e.max_index(out=pos[:, h, 0:8], in_max=top[:, h, 0:8], in_values=cand[:, h, :]),
                             reads=[cand_v[h], top_v[h]], writes=[pos_v[h]])
                    elif step == 2:
                        S.op("dve", lambda e, h=h: e.match_replace(out=cand[:, h, :], in_to_replace=top[:, h, 0:8], in_values=cand[:, h, :], imm_value=NEG),
                             reads=[cand_v[h], top_v[h], pos_v[h]], writes=[cand_v[h]])
                    elif step == 3:
                        S.op("dve", lambda e, h=h: e.max(out=top[:, h, 8:16], in_=cand[:, h, :]), reads=[cand_v[h]], writes=[top_v[h]])
                    else:
                        S.op("dve", lambda e, h=h: e.max_index(out=pos[:, h, 8:16], in_max=top[:, h, 8:16], in_values=cand[:, h, :]),
                             reads=[cand_v[h], top_v[h]], writes=[pos_v[h]])

            def softmax_step():
                S.op("dve", lambda e: e.tensor_tensor(out=ex[:, :, :], in0=top[:, :, :], in1=top[:, :, 0:1].broadcast_to([128, 8, 16]), op=ALU.subtract),
                     reads=top_v, writes=[ex])
                S.op("act", lambda e: e.activation(out=ex[:, :, :], in_=ex[:, :, :], func=AF.Exp), reads=[ex], writes=[ex])
                S.op("dve", lambda e: e.tensor_reduce(out=zs[:, :], in_=ex[:, :, :], axis=AX.X, op=ALU.add), reads=[ex], writes=[zs])
                S.op("dve", lambda e: e.reciprocal(out=zs[:, :], in_=zs[:, :]), reads=[zs], writes=[zs])
                S.op("dve", lambda e: e.tensor_tensor(out=gg[:, :, :], in0=ex[:, :, :], in1=zs[:, :].unsqueeze(2).broadcast_to([128, 8, 16]), op=ALU.mult),
                     reads=[ex, zs], writes=[gg])

            def decode_prep():
                S.op("dve", lambda e: e.tensor_single_scalar(out=au[:, :, :], in_=pos[:, :, :], scalar=4, op=ALU.arith_shift_right), reads=pos_v, writes=[au])
                S.op("dve", lambda e: e.tensor_single_scalar(out=bu[:, :, :], in_=pos[:, :, :], scalar=15, op=ALU.bitwise_and), reads=pos_v, writes=[bu])
                S.op("dve", lambda e: e.tensor_copy(out=af[:, :, :], in_=au[:, :, :]), reads=[au], writes=[af])
                S.op("dve", lambda e: e.tensor_copy(out=bf[:, :, :], in_=bu[:, :, :]), reads=[bu], writes=[bf])
                S.op("dve", lambda e: e.tensor_copy(out=sif[:, :, :], in_=si[:, :, :]), reads=si_v, writes=[sif])

            def decode(wi):
                sel, half_ = ((af, 0), (bf, 1))[wi]
                sif4 = sif.t[:, :, :].rearrange("p (h two) k -> p h two k", two=2)
                S.op("dve", lambda e: e.tensor_tensor(
                    out=oh[:, :, :], in0=sel[:, :, :].rearrange("p h k -> p (h k)").unsqueeze(2).broadcast_to([128, 128, 16]),
                    in1=iota16[:, :].unsqueeze(1).broadcast_to([128, 128, 16]), op=ALU.is_equal), reads=[sel, iota16], writes=scrA_all)
                S.op("dve", lambda e: e.tensor_tensor(
                    out=oh[:, :, :].rearrange("p (h k) a -> p h k a", k=16), in0=oh[:, :, :].rearrange("p (h k) a -> p h k a", k=16),
                    in1=sif4[:, :, half_, :].unsqueeze(2).broadcast_to([128, 8, 16, 16]), op=ALU.mult), reads=scrA_all + [sif], writes=scrA_all)
                S.op("dve", lambda e: e.tensor_reduce(out=IJ[:, wi, :], in_=oh[:, :, :], axis=AX.X, op=ALU.add), reads=scrA_all, writes=[IJ])

            def transp(tsl):
                def trp(e):
                    e.transpose(out=P[6][:, 0:128], in_=IJ[:, 0, :], identity=ident[:, :])
                    e.transpose(out=P[6][:, 128:256], in_=IJ[:, 1, :], identity=ident[:, :])
                    return e.transpose(out=P[6][:, 256:384], in_=gg[:, :, :].rearrange("p h k -> p (h k)"), identity=ident[:, :])
                S.op("pe", trp, reads=[IJ, gg, ident], writes=[P[6]])
                S.op("act", lambda e: e.copy(out=ijg_sb[:, :, tsl], in_=P[6][:, 0:384].rearrange("p (a t) -> p a t", t=128)),
                     reads=[P[6]], writes=[ijg_sb])

            for ts_ in range(2):
                tsl = slice(ts_ * 128, (ts_ + 1) * 128)
                for half in range(2):
                    steps.append(lambda tsl=tsl, half=half: scores(tsl, half))
                    for step in range(3):
                        steps.append(lambda half=half, step=step: topk_step(half, step))
                steps.append(idx_step)
                steps.append(cand_step)
                for step in range(5):
                    steps.append(lambda step=step: topk2_step(step))
                steps.append(softmax_step)
                steps.append(decode_prep)
                steps.append(lambda: decode(0))
                steps.append(lambda: decode(1))
                steps.append(lambda tsl=tsl: transp(tsl))
            steps.append(lambda: S.dma("sp", lambda e: e.dma_start(out=ijg[:, :, t0:t0 + TB], in_=ijg_sb[:, :, :]), reads=[ijg_sb], writes=[ijg_t[b]]))
            return steps

        def cast_steps():
            pieces = []
            for src, dst_t, dst in ((G["UT"], G["UTb_t"], G["UTb"]), (G["VV"], G["VVb_t"], G["VVb"])):
                for pc in range(131072 // CW2):
                    pieces.append((src, dst_t, dst, pc))

            def load(i):
                src, dst_t, dst, pc = pieces[i]
                fi = stg[i % 2]
                sl = slice(pc * CW2, (pc + 1) * CW2)
                S.dma("sp", lambda e: e.dma_start(out=fi[:, :], in_=src[:, sl]), reads=[src], writes=[fi])

            def piece(i):
                if i == 0:
                    load(0)
                if i + 1 < len(pieces):
                    load(i + 1)
                src, dst_t, dst, pc = pieces[i]
                fi = stg[i % 2]
                fo = cout[i % 2]
                sl = slice(pc * CW2, (pc + 1) * CW2)
                S.op("act", lambda e: e.copy(out=fo[:, :], in_=fi[:, :]), reads=[fi], writes=[fo])
                d = dst_t[(pc * CW2) // CAST_W]
                S.dma("act", lambda e: e.dma_start(out=dst[:, sl], in_=fo[:, :]), reads=[fo], writes=[d])
            return [(lambda i=i: piece(i)) for i in range(len(pieces))]

        with nc.Block() as block:
            S.dma("sp", lambda e: e.dma_start(out=small_sb[:], in_=G["small"][:]), reads=[G["small"]], writes=[small_sb])
            gmix.lw = gffn.lw = small_sb.lw
            S.dma("sp", lambda e: e.dma_start(out=ident[:], in_=G["ident_d"][:]), reads=[G["ident_d"]], writes=[ident])
            S.dma("sp", lambda e: e.dma_start(out=iota16[:], in_=G["iota_d"][:, 0:16]), reads=[G["iota_d"]], writes=[iota16])
            S.dma("sp", lambda e: e.dma_start(out=bits_sb[:], in_=G["bits_d"][:, :]), reads=[G["bits_d"]], writes=[bits_sb])
            S.dma("sp", lambda e: e.dma_start(out=edge[:], in_=G["edge_d"][:, :].rearrange("p (a g c) -> p a g c", a=2, g=4)),
                  reads=[G["edge_d"]], writes=[edge])
            S.op("pool", lambda e: e.memset(C.ones[:], 1.0), writes=[C.ones])
            S.op("pool", lambda e: e.memset(C.epsb[:], EPS), writes=[C.epsb])
            k = 0
            for c in range(8):
                _load_cast(S, stg, w_in_sb, (slice(None), c, slice(None)), G["w_in"],
                           (slice(c * 128, (c + 1) * 128), slice(None)), 2048, k); k += 1
            wsteps = []
            for c in range(8):
                wsteps.append(lambda c=c: _load_cast(S, stg, w_o_sb, (slice(None), c, slice(None)), G["w_o"],
                                                     (slice(c * 128, (c + 1) * 128), slice(None)), 1024, 8 + c))

            def _keys(hh):
                stt = stg[hh % 2]
                S.dma("sp", lambda e: e.dma_start(out=stt[:, 0:1024], in_=G["keysT"][:, hh * 1024:(hh + 1) * 1024]),
                      reads=[G["keysT"]], writes=[stt])
                S.op("dve", lambda e: e.tensor_copy(out=keys_sb[:, hh * 8:(hh + 1) * 8, :], in_=stt[:, 0:1024].rearrange("p (q k) -> p q k", k=128)),
                     reads=[stt], writes=[keys_sb])

            def _poolw():
                stt = stg[0]
                S.dma("sp", lambda e: e.dma_start(out=stt[:, 0:512], in_=G["poolw"][:, :]), reads=[G["poolw"]], writes=[stt])
                S.op("dve", lambda e: e.tensor_copy(out=poolw_sb[:, :, :], in_=stt[:, 0:512].rearrange("p (g d) -> p g d", d=128)),
                     reads=[stt], writes=[poolw_sb])
            wsteps.insert(0, _poolw)
            for c in range(8):
                wsteps.append(lambda c=c: _load_cast(S, stg, w_q_sb, (slice(None), c, slice(None)), G["w_q"],
                                                     (slice(c * 128, (c + 1) * 128), slice(None)), 2048, 16 + c))
            wsteps.append(lambda: _keys(0))
            wsteps.append(lambda: _keys(1))

            casts = cast_steps()
            nseg = nb + 2
            per = (len(casts) + nseg - 2) // (nseg - 1)
            cast_chunks = [[]] + [casts[i * per:(i + 1) * per] for i in range(nseg - 1)]
            for sg_ in range(nseg):
                lists = []
                if sg_ < nb:
                    lists.append(mixer_steps(sg_))
                if sg_ == 0:
                    lists.append(wsteps)
                if 0 <= sg_ - 1 < nb:
                    lists.append(q_steps(sg_ - 1))
                if 0 <= sg_ - 2 < nb:
                    lists.append(routing_steps(sg_ - 2))
                lists.append(cast_chunks[sg_])
                for stp in _merge(lists):
                    stp()
            S.flush(block)


TG = 16
JH = 64


def _phase2(nc, S, nb, G):
    from contextlib import ExitStack
    xn2T_v, h1T_v, pT_v, outT_v, ijg = G["xn2T_v"], G["h1T_v"], G["pT_v"], G["outT_v"], G["ijg"]
    UTb, VVb = G["UTb"], G["VVb"]
    UTb_t, VVb_t, h1T_t, xn2T_t, ijg_t = G["UTb_t"], G["VVb_t"], G["h1T_t"], G["xn2T_t"], G["ijg_t"]
    outT_t = _views(G["outT"], nb)
    with ExitStack() as st:
        def sb(name, shape, dt):
            return Tile(st.enter_context(nc.sbuf_tensor("b_" + name, shape, dt)), name)

        def psum(name):
            return Tile(st.enter_context(nc.psum_tensor("b_" + name, [128, 512], F32)), name)

        C = Ctx()
        w_pg_sb = sb("w_pg", [128, 8, 1024], BF16)
        w_pp_sb = sb("w_pp", [128, 2, 1024], BF16)
        small_sb = sb("small", [128, 48], F32)
        C.ones = sb("ones", [128, 128], BF16)
        C.epsb = sb("epsb", [128, 1], F32)
        iota_f = sb("iota_f", [128, 128], F32)
        iota3 = sb("iota3", [128, TG, 128], BF16)
        stg = [sb("stg%d" % i, [128, 1024], F32) for i in range(1)]
        Gh = [sb("G%d" % i, [128, JH, TB], BF16) for i in range(2)]
        NUB = 3
        ut = [sb("ut%d" % i, [128, 4096], BF16) for i in range(NUB)]
        vt = [sb("vt%d" % i, [128, 4096], BF16) for i in range(NUB)]
        xn2b = [sb("xn2b%d" % i, [128, 8, TB], BF16) for i in range(2)]
        h1b = [sb("h1b%d" % i, [128, 8, TB], F32) for i in range(2)]
        pTb = [sb("pTb%d" % i, [128, 2, TB], F32) for i in range(2)]
        pTbb = sb("pTbb", [128, 2, TB], BF16)
        ijgb = [sb("ijgb%d" % i, [128, 3, TB], BF16) for i in range(2)]
        OI = [sb("OI%d" % i, [128, TG, 128], BF16) for i in range(2)]
        OJ = [sb("OJ%d" % i, [128, TG, JH], BF16) for i in range(2)]
        sq = sb("sq", [128, 8, TB], BF16)
        rs = sb("rs", [128, TB], F32)
        xn3 = sb("xn3", [128, 8, TB], BF16)
        sg = [sb("sg%d" % i, [128, TB], F32) for i in range(2)]
        outb = sb("outb", [128, 8, TB], F32)
        acc = [psum("acc%d" % i) for i in range(4)]
        pa = [psum("pa%d" % i) for i in range(2)]
        pw = [psum("pw%d" % i) for i in range(2)]
        G_v = [_views(Gh[i], JH) for i in range(2)]
        Hd = [Tile(Gh[i].t, "Hd%d" % i) for i in range(2)]
        gple = Tile(small_sb.t[:, 16:24])
        gfin = Tile(small_sb.t[:, 24:32])

        with nc.Block() as block:
            S.dma("sp", lambda e: e.dma_start(out=small_sb[:], in_=G["small"][:]), reads=[G["small"]], writes=[small_sb])
            gple.lw = gfin.lw = small_sb.lw
            S.dma("sp", lambda e: e.dma_start(out=iota_f[:], in_=G["iota_d"][:, :]), reads=[G["iota_d"]], writes=[iota_f])
            S.op("pool", lambda e: e.memset(C.ones[:], 1.0), writes=[C.ones])
            S.op("pool", lambda e: e.memset(C.epsb[:], EPS), writes=[C.epsb])
            S.op("dve", lambda e: e.tensor_copy(out=iota3[:, :, :], in_=iota_f[:, :].unsqueeze(1).broadcast_to([128, TG, 128])),
                 reads=[iota_f], writes=[iota3])
            wl_thunks = []
            for c in range(8):
                wl_thunks.append(lambda c=c: _load_cast(S, stg, w_pg_sb, (slice(None), c, slice(None)), G["w_pg"],
                                                        (slice(c * 128, (c + 1) * 128), slice(None)), 1024, c))
            for c in range(2):
                wl_thunks.append(lambda c=c: _load_cast(S, stg, w_pp_sb, (slice(None), c, slice(None)), G["w_pp"],
                                                        (slice(c * 128, (c + 1) * 128), slice(None)), 1024, 8 + c))

            nload = [0, 0]

            def emit_loads(b):
                t0 = b * TB
                S.dma("sp", lambda e: e.dma_start(out=xn2b[b % 2][:, :, :], in_=xn2T_v[:, :, t0:t0 + TB]), reads=[xn2T_t[b]], writes=[xn2b[b % 2]])
                S.dma("sp", lambda e: e.dma_start(out=ijgb[b % 2][:, :, :], in_=ijg[:, :, t0:t0 + TB]), reads=[ijg_t[b]], writes=[ijgb[b % 2]])

            def emit_loads2(b):
                t0 = b * TB
                S.dma("sp", lambda e: e.dma_start(out=h1b[b % 2][:, :, :], in_=h1T_v[:, :, t0:t0 + TB]), reads=[h1T_t[b]], writes=[h1b[b % 2]])
                S.dma("sp", lambda e: e.dma_start(out=pTb[b % 2][:, :, :], in_=pT_v[:, :, t0:t0 + TB]), reads=[G["pT"]], writes=[pTb[b % 2]])

            prebuilt = set()

            def w_oct(kw, oc8):
                b, hf = kw // 2, kw % 2
                ij = ijgb[b % 2]
                gh = Gh[kw % 2]
                tg = oc8 // 2
                oi = OI[tg % 2]
                oj = OJ[tg % 2]
                def build(tgb):
                    oi_, oj_ = OI[tgb % 2], OJ[tgb % 2]
                    tk = slice(tgb * TG, (tgb + 1) * TG)
                    S.op("dve", lambda e: e.tensor_tensor(
                        out=oi_[:, :, :], in0=iota3[:, :, :], in1=ij[:, 0, tk].unsqueeze(2).broadcast_to([128, TG, 128]), op=ALU.is_equal),
                        reads=[iota3, ij], writes=[oi_])
                    S.op("dve", lambda e: e.tensor_tensor(
                        out=oj_[:, :, :], in0=iota3[:, :, hf * JH:(hf + 1) * JH], in1=ij[:, 1, tk].unsqueeze(2).broadcast_to([128, TG, JH]), op=ALU.is_equal),
                        reads=[iota3, ij], writes=[oj_])
                    S.op("pool", lambda e: e.tensor_tensor(
                        out=oj_[:, :, :], in0=oj_[:, :, :], in1=ij[:, 2, tk].unsqueeze(2).broadcast_to([128, TG, JH]), op=ALU.mult),
                        reads=[oj_, ij], writes=[oj_])
                if oc8 < 0:
                    build(0)
                    prebuilt.add(kw)
                    return
                if oc8 % 2 == 0:
                    if tg == 0 and kw not in prebuilt:
                        build(0)
                    if tg + 1 < TB // TG:
                        build(tg + 1)
                pp = pw[oc8 % 2]

                def mm(e, oi=oi, oj=oj, oc8=oc8, pp=pp):
                    ins = None
                    for tl in range(8):
                        tt = (oc8 % 2) * 8 + tl
                        ins = e.matmul(pp[:, :].rearrange("p (j t) -> p j t", t=8)[:, :, tl], lhsT=oi[:, tt, :], rhs=oj[:, tt, :],
                                       start=(tl == 0), stop=(tl == 7), skip_group_check=True)
                    return ins
                S.op("pe", mm, reads=[oi, oj], writes=[pp])
                tq = oc8 * 8
                S.op("dve", lambda e, pp=pp, tq=tq, gh=gh: e.tensor_tensor(
                    out=gh[:, :, tq:tq + 8],
                    in0=pp[:, :].rearrange("p (j t) -> p j t", t=8),
                    in1=gh[:, :, tq:tq + 8], op=ALU.mult),
                    reads=G_v[kw % 2] + [pp], writes=[Hd[kw % 2]], skip_same=True)

            def emit_A(ka, kw):
                b, hf = ka // 2, ka % 2
                if hf == 0:
                    emit_loads(b)
                else:
                    emit_loads2(b)
                xb = xn2b[b % 2]
                gh = Gh[ka % 2]
                oc8 = 0
                for gl in range(JH // 4):
                    gq = hf * (JH // 4) + gl
                    u = ut[nload[0] % NUB]
                    nload[0] += 1
                    S.dma("sp", lambda e, u=u, gq=gq: e.dma_start(out=u[:, :], in_=UTb[:, gq * 4096:(gq + 1) * 4096]),
                          reads=[UTb_t[gq]], writes=[u])
                    if wl_thunks and gl >= 2:
                        wl_thunks.pop(0)()
                    for jj in range(4):
                        jl = gl * 4 + jj
                        pp = pa[jl % 2]

                        def mm(e, u=u, jj=jj, pp=pp, xb=xb):
                            ins = None
                            for dc in range(8):
                                o = (jj * 8 + dc) * 128
                                ins = e.matmul(pp[:, 0:TB], lhsT=u[:, o:o + 128], rhs=xb[:, dc, :], start=(dc == 0), stop=(dc == 7))
                            return ins
                        S.op("pe", mm, reads=[u, xb], writes=[pp])
                        S.op("act", lambda e, jl=jl, pp=pp, gh=gh: e.activation(out=gh[:, jl, :], in_=pp[:, 0:TB], func=AF.Gelu_apprx_tanh),
                             reads=[pp], writes=[G_v[ka % 2][jl]])
                        if kw is not None and jl % 2 == 1:
                            w_oct(kw, oc8)
                            oc8 += 1

            def emit_W_only(kw):
                for oc8 in range(TB // 8):
                    w_oct(kw, oc8)

            def emit_V(kv, inter):
                b, hf = kv // 2, kv % 2
                gh = Gh[kv % 2]
                for gl in range(JH // 4):
                    if inter:
                        inter.pop(0)()
                    gq = hf * (JH // 4) + gl
                    v = vt[nload[1] % NUB]
                    nload[1] += 1
                    S.dma("sp", lambda e, v=v, gq=gq: e.dma_start(out=v[:, :], in_=VVb[:, gq * 4096:(gq + 1) * 4096]),
                          reads=[VVb_t[gq]], writes=[v])
                    for jj in range(4):
                        jl = gl * 4 + jj
                        j = hf * JH + jl

                        def mm(e, v=v, jj=jj, j=j, jl=jl, gh=gh):
                            ins = None
                            for dc in range(8):
                                o = jj * 1024 + dc * 128
                                ins = e.matmul(acc[dc // 2][:, (dc % 2) * TB:(dc % 2 + 1) * TB], lhsT=v[:, o:o + 128], rhs=gh[:, jl, :],
                                               start=(j == 0 and dc % 2 == 0), stop=(j == 127), skip_group_check=True)
                            return ins
                        S.op("pe", mm, reads=[v, G_v[kv % 2][jl], Hd[kv % 2]], writes=acc)

            def norm_pre(h):
                for c in range(8):
                    if c % 2 == 0:
                        S.op("act", lambda e, c=c: e.activation(out=sq[:, c, :], in_=h[:, c, :], func=AF.Square), reads=[h], writes=[sq])
                    else:
                        S.op("pool", lambda e, c=c: e.tensor_tensor(out=sq[:, c, :], in0=h[:, c, :], in1=h[:, c, :], op=ALU.mult),
                             reads=[h], writes=[sq])

            def stage_E1a(b):
                h = h1b[b % 2]
                for kk in range(4):
                    S.op("dve", lambda e, kk=kk: e.tensor_tensor(
                        out=h[:, 2 * kk:2 * kk + 2, :], in0=acc[kk][:, :].rearrange("p (c t) -> p c t", t=TB),
                        in1=h[:, 2 * kk:2 * kk + 2, :], op=ALU.add), reads=[acc[kk], h], writes=[h])
                S.op("pool", lambda e: e.tensor_copy(out=pTbb[:, :, :], in_=pTb[b % 2][:, :, :]), reads=[pTb[b % 2]], writes=[pTbb])
                norm_pre(h)

            def norm_post_a():
                def mm(e):
                    ins = None
                    for c in range(8):
                        ins = e.matmul(pa[0][:, 0:TB], lhsT=C.ones[:, :], rhs=sq[:, c, :], start=(c == 0), stop=(c == 7))
                    return ins
                S.op("pe", mm, reads=[sq, C.ones], writes=[pa[0]])
                S.op("act", lambda e: e.activation(out=rs[:, :], in_=pa[0][:, 0:TB], func=AF.Sqrt, bias=C.epsb[:, 0:1], scale=1.0 / D),
                     reads=[pa[0], C.epsb], writes=[rs])
                S.op("dve", lambda e: e.reciprocal(out=rs[:, :], in_=rs[:, :]), reads=[rs], writes=[rs])

            def norm_post_b(h, g_sb, dst):
                for c in range(8):
                    S.op("dve", lambda e, c=c: e.scalar_tensor_tensor(out=dst[:, c, :], in0=h[:, c, :], scalar=g_sb[:, c:c + 1], in1=rs[:, :],
                                                                      op0=ALU.mult, op1=ALU.mult), reads=[h, g_sb, rs], writes=[dst])

            def ple_oc(b, oc):
                h = h1b[b % 2]
                pp = (pa + pw)[oc % 4]
                sgt = sg[oc % 2]

                def mm(e):
                    for c in range(8):
                        e.matmul(pp[:, 0:TB], lhsT=w_pg_sb[:, c, oc * 128:(oc + 1) * 128], rhs=xn3[:, c, :], start=(c == 0), stop=(c == 7))
                    ins = None
                    for c in range(2):
                        ins = e.matmul(pp[:, TB:2 * TB], lhsT=w_pp_sb[:, c, oc * 128:(oc + 1) * 128], rhs=pTbb[:, c, :],
                                       start=(c == 0), stop=(c == 1))
                    return ins
                S.op("pe", mm, reads=[w_pg_sb, w_pp_sb, xn3, pTbb], writes=[pp])
                S.op("act", lambda e: e.activation(out=sgt[:, :], in_=pp[:, 0:TB], func=AF.Sigmoid), reads=[pp], writes=[sgt])
                S.op("dve", lambda e: e.tensor_tensor(out=sgt[:, :], in0=pp[:, TB:2 * TB], in1=sgt[:, :], op=ALU.mult), reads=[pp, sgt], writes=[sgt])
                S.op("dve", lambda e: e.tensor_tensor(out=h[:, oc, :], in0=h[:, oc, :], in1=sgt[:, :], op=ALU.add), reads=[h, sgt], writes=[h])

            toks = []

            def store_out(b):
                t0 = b * TB
                toks.append(S.dma("sp", lambda e: e.dma_start(out=outT_v[:, :, t0:t0 + TB], in_=outb[:, :, :]), reads=[outb], writes=[outT_t[b]]))

            def epilogue_thunks(b):
                h = h1b[b % 2]
                th = [norm_post_a, lambda: norm_post_b(h, gple, xn3)]
                for oc in range(8):
                    th.append(lambda oc=oc: ple_oc(b, oc))
                th.append(lambda: norm_pre(h))
                th.append(norm_post_a)
                th.append(lambda: norm_post_b(h, gfin, outb))
                th.append(lambda: store_out(b))
                return th

            pending = []

            nk = 2 * nb
            emit_A(0, None)
            for kk_ in range(1, nk + 1):
                if kk_ < nk:
                    emit_A(kk_, kk_ - 1)
                else:
                    emit_W_only(kk_ - 1)
                if kk_ < nk:
                    w_oct(kk_, -1)
                emit_V(kk_ - 1, pending)
                if (kk_ - 1) % 2 == 1:
                    bdone = (kk_ - 1) // 2
                    stage_E1a(bdone)
                    pending.extend(epilogue_thunks(bdone))
            while pending:
                pending.pop(0)()
            S.wait_all("sp", toks)
            S.flush(block)


def _prep_shared(inp, nb):
    f = np.float32
    sh = {}
    sh["w_in"] = np.ascontiguousarray(inp["w_in"][0], dtype=f)
    sh["w_o"] = np.ascontiguousarray(inp["w_o"][0], dtype=f)
    sh["w_q"] = np.ascontiguousarray(inp["w_q"][0], dtype=f)
    sh["w_pg"] = np.ascontiguousarray(inp["w_ple_gate"][0], dtype=f)
    sh["w_pp"] = np.ascontiguousarray(inp["w_ple_proj"][0], dtype=f)
    sh["keysT"] = np.ascontiguousarray(np.asarray(inp["sub_keys"][0]).reshape(16, 128, 128).transpose(2, 0, 1).reshape(128, 2048), dtype=f)
    sh["poolw"] = np.ascontiguousarray(np.asarray(inp["pool_w"][0]).transpose(1, 0, 2).reshape(128, 512), dtype=f)
    small = np.zeros((128, 48), dtype=f)
    small[:, 0:8] = np.asarray(inp["g_mix"][0]).reshape(8, 128).T
    small[:, 8:16] = np.asarray(inp["g_ffn"][0]).reshape(8, 128).T
    small[:, 16:24] = np.asarray(inp["g_ple"][0]).reshape(8, 128).T
    small[:, 24:32] = np.asarray(inp["g_final"]).reshape(8, 128).T
    small[:, 32:44] = np.asarray(inp["conv_w"][0]).T.reshape(4, 128, 3).transpose(1, 0, 2).reshape(128, 12)
    small[:, 44:48] = np.asarray(inp["pool_scale"][0]).reshape(4, 128).T
    sh["small"] = small
    sh["UT"] = np.ascontiguousarray(np.asarray(inp["expert_u"][0]).reshape(128, 128, 8, 128).transpose(3, 1, 2, 0).reshape(128, 131072), dtype=f)
    sh["VV"] = np.ascontiguousarray(np.asarray(inp["expert_v"][0]).reshape(128, 131072), dtype=f)
    sh["ident"] = np.eye(128, dtype=f)
    sh["iota"] = np.tile(np.arange(128, dtype=f), (128, 1))
    bits = np.zeros((128, 256), dtype=np.uint32)
    bits[:, 0:128] = np.uint32(0xFFFFFF80)
    bits[:, 128:256] = np.arange(128, dtype=np.uint32)[None, :]
    sh["bits"] = bits
    seq = nb * TB
    edge = np.ones((2, 4, 8), dtype=f)
    for g in range(4):
        w = 2 << g
        for c in range(8):
            for a, t in ((0, c), (1, seq - 8 + c)):
                lo = min(max(t - w // 2, 0), seq)
                hi = min(max(t + w // 2, 0), seq)
                edge[a, g, c] = w / float(hi - lo)
    sh["edge"] = np.tile(edge.reshape(1, 64), (128, 1)).astype(f)
    return sh


def _prep_core(inp, sh, b, nb):
    seq = nb * TB
    m = dict(sh)
    m["xT"] = np.ascontiguousarray(np.asarray(inp["x"][b, :seq]).T, dtype=np.float32)
    m["pT"] = np.ascontiguousarray(np.asarray(inp["p"][0, b, :seq]).T, dtype=np.float32)
    return m


def kernel(**inputs):
    nb = SEQ // TB
    sh = _prep_shared(inputs, nb)
    in_maps = [_prep_core(inputs, sh, b, nb) for b in range(NCORES)]
    nc = bass.Bass("TRN2", target_bir_lowering=False)
    build_program(nc, nb, debug=False)
    res = run_bass_kernel_spmd(nc, in_maps, core_ids=list(range(NCORES)))
    out = np.stack([np.ascontiguousarray(np.asarray(r["outT"]).T) for r in res.results], axis=0)
    return out.astype(np.float32)
```
